# Optimizing a Trainium2 kernel written in Bass

```python
import jax, jax.numpy as jnp
from jax import lax
import numpy as np

D_MODEL = 1024
BATCH = 2
SEQ = 8192
DEPTH = 1

D_MIX = D_MODEL
HEAD_DIM = 64
N_Q_HEADS = 8
N_KV_HEADS = 2
Q_PER_KV = N_Q_HEADS // N_KV_HEADS
D_ATT = N_Q_HEADS * HEAD_DIM
D_CONV = D_MIX - D_ATT
KV_W = N_KV_HEADS * HEAD_DIM
N_BRANCH = 3
CONV_TAPS = 3
ROT_DIM = HEAD_DIM // 4
ROPE_THETA = 500000.0
CMP_BLOCK = 32
CMP_STRIDE = 16
CMP_HIDDEN = 256
SLC_BLOCK = 64
N_SELECT = 16
WINDOW = 512
Q_BLOCK = 128
D_FF = 2816
D_PLE = 256
EPS = 1e-6
NEG = -1e30
D_IN = D_ATT + 6 * KV_W + N_BRANCH * N_Q_HEADS + 3 * D_CONV

kernel_name = "hybrid_nsa_shortconv_convglu_ple"


def rms_norm(x, g):
    xf = x.astype(jnp.float32)
    y = xf * lax.rsqrt(jnp.mean(xf * xf, axis=-1, keepdims=True) + EPS)
    return (y * g.astype(jnp.float32)).astype(x.dtype)


def partial_rope(x, pos):
    half = ROT_DIM // 2
    inv_freq = ROPE_THETA ** (-jnp.arange(half, dtype=jnp.float32) * 2.0 / ROT_DIM)
    ang = pos.astype(jnp.float32)[:, None] * inv_freq[None, :]
    ang = ang.reshape(ang.shape[:1] + (1,) * (x.ndim - 3) + (half,))
    cos, sin = jnp.cos(ang), jnp.sin(ang)
    xf = x.astype(jnp.float32)
    x1, x2, rest = xf[..., :half], xf[..., half:ROT_DIM], xf[..., ROT_DIM:]
    out = jnp.concatenate([x1 * cos - x2 * sin, x2 * cos + x1 * sin, rest], axis=-1)
    return out.astype(x.dtype)


def causal_dwconv(u, w):
    c = u.shape[-1]
    return lax.conv_general_dilated(u, w[:, None, :].astype(u.dtype), window_strides=(1,),
                                    padding=[(CONV_TAPS - 1, 0)],
                                    dimension_numbers=('NWC', 'WIO', 'NWC'),
                                    feature_group_count=c)


def masked_softmax(s, valid):
    p = jax.nn.softmax(jnp.where(valid, s, NEG), axis=-1)
    return jnp.where(valid, p, 0.0)


def compress_blocks(tok, pe, w1, w2):
    t = tok.shape[1]
    n_cmp = (t - CMP_BLOCK) // CMP_STRIDE + 1
    idx = CMP_STRIDE * jnp.arange(n_cmp)[:, None] + jnp.arange(CMP_BLOCK)[None, :]
    blk = tok[:, idx] + pe[None, None, :, None, :]
    hid = jax.nn.gelu(jnp.einsum('bnlgd,ldh->bngh', blk, w1))
    return jnp.einsum('bngh,hd->bngd', hid, w2)


def cmp_to_slc_weights(n_cmp, n_slc):
    cs = CMP_STRIDE * jnp.arange(n_cmp)[:, None]
    ss = SLC_BLOCK * jnp.arange(n_slc)[None, :]
    ov = jnp.clip(jnp.minimum(cs + CMP_BLOCK, ss + SLC_BLOCK) - jnp.maximum(cs, ss), 0)
    return ov.astype(jnp.float32) / CMP_BLOCK


def nsa_attention(q, kc, vc, ks, vs, kw, vw, gates, qn_g, kn_g, pe_k, pe_v, w_ck1, w_ck2, w_cv1, w_cv2):
    b, t = q.shape[:2]
    pos = jnp.arange(t)
    scale = HEAD_DIM ** -0.5
    q = partial_rope(rms_norm(q, qn_g), pos)
    kcmp = compress_blocks(kc, pe_k, w_ck1, w_ck2)
    vcmp = compress_blocks(vc, pe_v, w_cv1, w_cv2)
    n_cmp = kcmp.shape[1]
    cmp_end = CMP_STRIDE * jnp.arange(n_cmp) + CMP_BLOCK - 1
    kcmp = partial_rope(rms_norm(kcmp, kn_g[0]), cmp_end)
    n_slc = t // SLC_BLOCK
    n_sel = min(N_SELECT, n_slc)
    ks = partial_rope(rms_norm(ks, kn_g[1]), pos)
    ks_blk = ks.reshape(b, n_slc, SLC_BLOCK, N_KV_HEADS, HEAD_DIM).transpose(0, 3, 1, 2, 4)
    vs_blk = vs.reshape(b, n_slc, SLC_BLOCK, N_KV_HEADS, HEAD_DIM).transpose(0, 3, 1, 2, 4)
    cmp2slc = cmp_to_slc_weights(n_cmp, n_slc)
    blk_ids = jnp.arange(n_slc)
    kw = partial_rope(rms_norm(kw, kn_g[2]), pos)
    kw_pad = jnp.pad(kw, ((0, 0), (WINDOW, 0), (0, 0), (0, 0)))
    vw_pad = jnp.pad(vw, ((0, 0), (WINDOW, 0), (0, 0), (0, 0)))
    gather_blocks = jax.vmap(jax.vmap(lambda blocks, idx: blocks[idx]))

    def block_fn(start):
        tq = start + jnp.arange(Q_BLOCK)
        qb = lax.dynamic_slice_in_dim(q, start, Q_BLOCK, axis=1)
        gb = lax.dynamic_slice_in_dim(gates, start, Q_BLOCK, axis=1)
        s_c = jnp.einsum('bqgrd,bngd->bgrqn', qb, kcmp).astype(jnp.float32) * scale
        p_c = masked_softmax(s_c, cmp_end[None, :] <= tq[:, None])
        o_c = jnp.einsum('bgrqn,bngd->bqgrd', p_c.astype(vcmp.dtype), vcmp)
        imp = jnp.einsum('bgrqn,ns->bgqs', p_c, cmp2slc)
        cur = tq // SLC_BLOCK
        forced = (blk_ids[None, :] == 0) | (blk_ids[None, :] == cur[:, None]) | (blk_ids[None, :] == cur[:, None] - 1)
        future = blk_ids[None, :] * SLC_BLOCK > tq[:, None]
        imp = jnp.where(forced, 1e9, jnp.where(future, -1e9, imp))
        _, sel = lax.top_k(imp, n_sel)
        k_sel = gather_blocks(ks_blk, sel)
        v_sel = gather_blocks(vs_blk, sel)
        kpos = sel[..., None] * SLC_BLOCK + jnp.arange(SLC_BLOCK)
        valid_s = (kpos <= tq[:, None, None])[:, :, None]
        s_s = jnp.einsum('bqgrd,bgqnkd->bgrqnk', qb, k_sel).astype(jnp.float32) * scale
        flat = s_s.shape[:4] + (n_sel * SLC_BLOCK,)
        p_s = masked_softmax(s_s.reshape(flat), valid_s.reshape(b, N_KV_HEADS, 1, Q_BLOCK, n_sel * SLC_BLOCK)).reshape(s_s.shape)
        o_s = jnp.einsum('bgrqnk,bgqnkd->bqgrd', p_s.astype(v_sel.dtype), v_sel)
        kwb = lax.dynamic_slice_in_dim(kw_pad, start, WINDOW + Q_BLOCK, axis=1)
        vwb = lax.dynamic_slice_in_dim(vw_pad, start, WINDOW + Q_BLOCK, axis=1)
        kpos_w = start - WINDOW + jnp.arange(WINDOW + Q_BLOCK)
        valid_w = (kpos_w[None, :] <= tq[:, None]) & (kpos_w[None, :] > tq[:, None] - WINDOW) & (kpos_w[None, :] >= 0)
        s_w = jnp.einsum('bqgrd,bkgd->bgrqk', qb, kwb).astype(jnp.float32) * scale
        p_w = masked_softmax(s_w, valid_w)
        o_w = jnp.einsum('bgrqk,bkgd->bqgrd', p_w.astype(vwb.dtype), vwb)
        return gb[..., 0:1] * o_c + gb[..., 1:2] * o_s + gb[..., 2:3] * o_w

    starts = jnp.arange(t // Q_BLOCK) * Q_BLOCK
    out = lax.map(block_fn, starts)
    return out.transpose(1, 0, 2, 3, 4, 5).reshape(b, t, D_ATT)


def hybrid_layer(h, p_l, ln_mix_g, w_in, qn_g, kn_g, pe_k, pe_v, w_ck1, w_ck2, w_cv1, w_cv2,
                 conv_w, on_att_g, on_conv_g, w_o, ln_ffn_g, w_up, ffn_conv_w, ffn_conv_b, w_down,
                 ln_ple_g, w_pg, w_pe):
    b, t, _ = h.shape
    u = rms_norm(h, ln_mix_g) @ w_in
    offs = np.cumsum([D_ATT] + [KV_W] * 6 + [N_BRANCH * N_Q_HEADS, D_CONV, D_CONV]).tolist()
    q, kc, vc, ks, vs, kw, vw, g_logit, cb, cc, cx = jnp.split(u, offs, axis=-1)
    q = q.reshape(b, t, N_KV_HEADS, Q_PER_KV, HEAD_DIM)
    kv_shape = (b, t, N_KV_HEADS, HEAD_DIM)
    gates = jax.nn.sigmoid(g_logit).reshape(b, t, N_KV_HEADS, Q_PER_KV, N_BRANCH)
    o_att = nsa_attention(q, kc.reshape(kv_shape), vc.reshape(kv_shape), ks.reshape(kv_shape),
                          vs.reshape(kv_shape), kw.reshape(kv_shape), vw.reshape(kv_shape), gates,
                          qn_g, kn_g, pe_k, pe_v, w_ck1, w_ck2, w_cv1, w_cv2)
    o_conv = cb * causal_dwconv(cc * cx, conv_w)
    mixed = jnp.concatenate([rms_norm(o_att, on_att_g), rms_norm(o_conv, on_conv_g)], axis=-1)
    h = h + mixed @ w_o
    a = rms_norm(h, ln_ffn_g) @ w_up
    gate, up = jnp.split(a, [D_FF], axis=-1)
    gate = causal_dwconv(gate, ffn_conv_w) + ffn_conv_b
    h = h + (jax.nn.silu(gate) * up) @ w_down
    h = h + jax.nn.sigmoid(rms_norm(h, ln_ple_g) @ w_pg) * (p_l @ w_pe)
    return h


def setup_inputs(seed: int = 0) -> dict:
    key = jax.random.key(seed)
    ks = jax.random.split(key, 32)
    f32 = jnp.float32

    def nrm(k, shape, scale):
        return jax.random.normal(k, shape, f32) * scale

    def gain(k, shape):
        return 1.0 + 0.01 * jax.random.normal(k, shape, f32)

    L = DEPTH
    return {
        'x': nrm(ks[0], (BATCH, SEQ, D_MODEL), 1.0),
        'p': nrm(ks[1], (DEPTH, BATCH, SEQ, D_PLE), 1.0),
        'ln_mix_g': gain(ks[2], (L, D_MODEL)),
        'w_in': nrm(ks[3], (L, D_MODEL, D_IN), D_MODEL ** -0.5),
        'qn_g': gain(ks[4], (L, HEAD_DIM)),
        'kn_g': gain(ks[5], (L, N_BRANCH, HEAD_DIM)),
        'pe_k': nrm(ks[6], (L, CMP_BLOCK, HEAD_DIM), 0.2),
        'pe_v': nrm(ks[7], (L, CMP_BLOCK, HEAD_DIM), 0.2),
        'w_ck1': nrm(ks[8], (L, CMP_BLOCK, HEAD_DIM, CMP_HIDDEN), (CMP_BLOCK * HEAD_DIM) ** -0.5),
        'w_ck2': nrm(ks[9], (L, CMP_HIDDEN, HEAD_DIM), CMP_HIDDEN ** -0.5),
        'w_cv1': nrm(ks[10], (L, CMP_BLOCK, HEAD_DIM, CMP_HIDDEN), (CMP_BLOCK * HEAD_DIM) ** -0.5),
        'w_cv2': nrm(ks[11], (L, CMP_HIDDEN, HEAD_DIM), CMP_HIDDEN ** -0.5),
        'conv_w': nrm(ks[12], (L, CONV_TAPS, D_CONV), CONV_TAPS ** -0.5),
        'on_att_g': gain(ks[13], (L, D_ATT)),
        'on_conv_g': gain(ks[14], (L, D_CONV)),
        'w_o': nrm(ks[15], (L, D_MIX, D_MODEL), D_MIX ** -0.5),
        'ln_ffn_g': gain(ks[16], (L, D_MODEL)),
        'w_up': nrm(ks[17], (L, D_MODEL, 2 * D_FF), D_MODEL ** -0.5),
        'ffn_conv_w': nrm(ks[18], (L, CONV_TAPS, D_FF), CONV_TAPS ** -0.5),
        'ffn_conv_b': nrm(ks[19], (L, D_FF), 0.01),
        'w_down': nrm(ks[20], (L, D_FF, D_MODEL), D_FF ** -0.5),
        'ln_ple_g': gain(ks[21], (L, D_MODEL)),
        'w_pg': nrm(ks[22], (L, D_MODEL, D_MODEL), D_MODEL ** -0.5),
        'w_pe': nrm(ks[23], (L, D_PLE, D_MODEL), D_PLE ** -0.5),
    }


def reference(x, p, ln_mix_g, w_in, qn_g, kn_g, pe_k, pe_v, w_ck1, w_ck2, w_cv1, w_cv2,
              conv_w, on_att_g, on_conv_g, w_o, ln_ffn_g, w_up, ffn_conv_w, ffn_conv_b, w_down,
              ln_ple_g, w_pg, w_pe):
    h = x
    for i in range(DEPTH):
        h = hybrid_layer(h, p[i], ln_mix_g[i], w_in[i], qn_g[i], kn_g[i], pe_k[i], pe_v[i],
                         w_ck1[i], w_ck2[i], w_cv1[i], w_cv2[i], conv_w[i], on_att_g[i], on_conv_g[i],
                         w_o[i], ln_ffn_g[i], w_up[i], ffn_conv_w[i], ffn_conv_b[i], w_down[i],
                         ln_ple_g[i], w_pg[i], w_pe[i])
    return h
```

```python
from contextlib import ExitStack
import os
import numpy as np
import concourse.bass as bass
import concourse.mybir as mybir
from concourse.bass_utils import run_bass_kernel_spmd

F32 = mybir.dt.float32
BF16 = mybir.dt.bfloat16
AF = mybir.ActivationFunctionType
ALU = mybir.AluOpType
AX = mybir.AxisListType

D = 1024
SEQ = 8192
NT = 64
NSLOT = 16
DFF = 2816
NF = 22
EPS = 1e-6
BIG = 30000.0
ROPE_THETA = 500000.0

PP_GMIX = 0
PP_GFFN = 8
PP_GPLE = 16
PP_OAG = 24
PP_OCG = 28
PP_QG = 32
PP_KG = 33
PP_CW = 36
PP_FW = 48
PP_FB = 114
PP_PAD = 136
NPP = 140


class _Stop(Exception):
    pass


class Trk:
    B = 2000

    def __init__(s, nc, es):
        s.nc, s.es = nc, es
        s.E = {'pe': nc.tensor, 'act': nc.scalar, 'dve': nc.vector, 'pool': nc.gpsimd, 'sp': nc.sync}
        s.seq = {e: 0 for e in s.E}
        s.esems = {e: [] for e in s.E}
        s.know = {}
        s.gi = {}
        s.gcount = 0
        s.clock = {}
        s.lastw = {}
        s.readers = {}
        s.dsem = {}
        s.dcnt = {}
        s.nsem = 0

    def _newsem(s, name):
        s.nsem += 1
        return s.es.enter_context(s.nc.semaphore(name))

    def _esem(s, e, seq):
        k = (seq - 1) // s.B
        while len(s.esems[e]) <= k:
            s.esems[e].append(s._newsem(f"s_{e}{len(s.esems[e])}"))
        return s.esems[e][k], (seq - 1) % s.B + 1

    def _known(s, me):
        return s.know.setdefault(me, {})

    def _merge(s, me, clock):
        kn = s._known(me)
        for d, v in clock.items():
            if v > kn.get(d, 0):
                kn[d] = v

    def _sync(s, me, r, w):
        deps = []
        for x in r:
            lw = s.lastw.get(x)
            if lw is not None:
                deps.append(lw)
            if x.startswith('ps'):
                for rd in s.readers.get(x, ()):
                    if rd[0] == 'eng' and rd[1] != me:
                        deps.append(rd)
        for x in w:
            lw = s.lastw.get(x)
            if lw is not None:
                deps.append(lw)
            deps.extend(s.readers.get(x, ()))
        kn = s._known(me)
        need = {}
        for d in deps:
            if d[0] == 'eng':
                _, e, q = d
                if e == me and me == 'pe':
                    continue
                dim = e
            else:
                _, k, q = d
                dim = 'dma:' + k
            if q > kn.get(dim, 0):
                need[dim] = max(need.get(dim, 0), q)
        waits = []
        for dim, q in sorted(need.items(), key=lambda t: -s.gi.get(t, 0)):
            if q <= kn.get(dim, 0):
                continue
            if dim.startswith('dma:'):
                sem, v = s.dsem[dim[4:]], q
            else:
                sem, v = s._esem(dim, q)
            waits.append((sem, v))
            kn[dim] = q
            s._merge(me, s.clock.get((dim, q), {}))
        return waits

    def op(s, me, fn, r=(), w=(), inc=True):
        if not inc:
            for sem, v in s._sync(me, r, w):
                s.E[me].wait_ge(sem, v)
            ins = fn(s.E[me])
            tag = ('eng', me, s.seq[me] + 1)
            for x in r:
                s.readers.setdefault(x, []).append(tag)
            for x in w:
                s.lastw[x] = tag
                s.readers[x] = []
            return ins
        s.nops = getattr(s, 'nops', 0) + 1
        lim = int(os.environ.get('KLIMIT', '0'))
        if lim and s.nops > lim:
            s.barrier()
            raise _Stop()
        waits = s._sync(me, r, w)
        eng = s.E[me]
        attach = None
        if waits and me != 'pe':
            attach = waits.pop()
        for sem, v in waits:
            eng.wait_ge(sem, v)
        ins = fn(eng)
        if attach is not None:
            ins._wait_ge(attach[0], attach[1])
        s.seq[me] += 1
        q = s.seq[me]
        sem, _ = s._esem(me, q)
        ins.then_inc(sem, 1)
        ck = dict(s._known(me))
        ck[me] = q
        s.clock[(me, q)] = ck
        s.gcount += 1
        s.gi[(me, q)] = s.gcount
        tag = ('eng', me, q)
        for x in r:
            s.readers.setdefault(x, []).append(tag)
        for x in w:
            s.lastw[x] = tag
            s.readers[x] = []
        return ins

    def dma(s, q, out, in_, r=(), w=(), key=None):
        waits = s._sync(q, r, w)
        for sem, v in waits:
            s.E[q].wait_ge(sem, v)
        if key not in s.dsem:
            s.dsem[key] = s._newsem("d_" + key)
            s.dcnt[key] = 0
        ins = s.E[q].dma_start(out=out, in_=in_)
        ins.then_inc(s.dsem[key], 16)
        s.dcnt[key] += 16
        ck = dict(s._known(q))
        ck['dma:' + key] = s.dcnt[key]
        s.clock[('dma:' + key, s.dcnt[key])] = ck
        s.gcount += 1
        s.gi[('dma:' + key, s.dcnt[key])] = s.gcount
        tag = ('dma', key, s.dcnt[key])
        for x in r:
            s.readers.setdefault(x, []).append(tag)
        for x in w:
            s.lastw[x] = tag
            s.readers[x] = []
        return ins

    def barrier(s):
        for me, eng in s.E.items():
            kn = s._known(me)
            for e in s.E:
                if e != me and s.seq[e] > kn.get(e, 0):
                    sem, v = s._esem(e, s.seq[e])
                    eng.wait_ge(sem, v)
                    kn[e] = s.seq[e]
            for k, cnt in s.dcnt.items():
                if cnt > kn.get('dma:' + k, 0):
                    eng.wait_ge(s.dsem[k], cnt)
                    kn['dma:' + k] = cnt

    def finish(s, q, keys):
        for k in keys:
            s.E[q].wait_ge(s.dsem[k], s.dcnt[k])


def build_nc(stop=None):
    nc = bass.Bass("TRN2", target_bir_lowering=False)
    try:
        _build(nc, stop)
    except _Stop:
        pass
    return nc


def _build(nc, stop):

    def din(name, shape, dt=F32):
        return nc.dram_tensor(name, list(shape), dt, kind="ExternalInput").ap()

    xts = din("xts", [NT, 128, 8, 128])
    ptd = din("ptd", [NSLOT, 128, 2, 128])
    cosT = din("cosT", [128, SEQ]); sinT = din("sinT", [128, SEQ])
    cosC = din("cosC", [128, 512]); sinC = din("sinC", [128, 512])
    w_in = din("w_in", [D, 2840])
    w_o = din("w_o", [D, D]); w_up = din("w_up", [D, 2 * DFF]); w_down = din("w_down", [DFF, D])
    w_pg = din("w_pg", [D, D]); w_pe = din("w_pe", [256, D])
    w1d = [din("w1k", [128, 32 * 256]), din("w1v", [128, 32 * 256])]
    w2d = [din("w2k", [128, 2 * 64]), din("w2v", [128, 2 * 64])]
    ped = [din("pek", [128, 32]), din("pev", [128, 32])]
    ppd = din("ppd", [128, NPP])
    tcd = din("tcd", [128, 128]); tbd = din("tbd", [128, 128])
    ccd = din("ccd", [2 * NSLOT, 4, 128, 128])
    ibd = din("ibd", [2 * NSLOT, 128, 128])
    indd = din("indd", [64, SEQ])
    cmd = din("cmd", [128, 4, 128])
    rotd = din("rotd", [128, 128]); obdd = din("obdd", [128, 128])
    outT = nc.dram_tensor("outT", [NSLOT, 128, 8, 128], F32, kind="ExternalOutput").ap()
    hmd = nc.dram_tensor("hmd", [NSLOT, 128, 8, 130], F32, kind="Internal").ap()
    hn3d = nc.dram_tensor("hn3d", [NSLOT, 128, 8, 130], BF16, kind="Internal").ap()

    with ExitStack() as es:
        K = Trk(nc, es)

        def mk(stack):
            def sb(name, shape, dt=F32):
                return stack.enter_context(nc.sbuf_tensor(name, list(shape), dt))
            return sb
        sb = mk(es)

        ps = [es.enter_context(nc.psum_tensor(f"ps{i}", [128, 512], F32)) for i in range(8)]
        psn = [f"ps{i}" for i in range(8)]
        rr = {}

        def nxt(lo=0, hi=8):
            n = hi - lo
            i = rr.get((lo, hi), 0)
            rr[(lo, hi)] = i + 1
            return lo + i % n

        dkeys = []

        def dump(name, ap2d, res):
            P, n = ap2d.shape[0], ap2d.shape[1]
            d = nc.dram_tensor("dbg_" + name, [P, n], F32, kind="ExternalOutput").ap()
            c = 0
            while c < n:
                ce = min(c + 2048, n)
                K.dma('pool', d[:, c:ce], ap2d[:, c:ce], r=[res], w=['dbg_' + name], key='dbg_' + name)
                c = ce
            dkeys.append('dbg_' + name)

        def stop_here():
            K.finish('pool', dkeys)
            raise _Stop()

        identf = sb("identf", [128, 128]); identb = sb("identb", [128, 128], BF16)
        onesb = sb("onesb", [128, 128], BF16); obd = sb("obd", [128, 128], BF16)
        rot = sb("rot", [128, 128], BF16)
        pp = sb("pp", [128, NPP]); qgs = sb("qgs", [128, 1]); epsb = sb("epsb", [128, 1])
        tc_b = sb("tc_b", [128, 128], BF16); tb_b = sb("tb_b", [128, 128], BF16)
        K.op('pool', lambda e: e.memset(identf[:], 0.0), w=['identf'])
        K.op('pool', lambda e: e.affine_select(out=identf[:], in_=identf[:], compare_op=ALU.not_equal, fill=1.0,
                                               base=0, pattern=[[-1, 128]], channel_multiplier=1),
             r=['identf'], w=['identf'])
        K.op('pool', lambda e: e.tensor_copy(out=identb[:], in_=identf[:]), r=['identf'], w=['identb'])
        K.op('pool', lambda e: e.memset(onesb[:], 1.0), w=['onesb'])
        K.op('pool', lambda e: e.memset(epsb[:], EPS), w=['epsb'])
        K.dma('sp', pp[:], ppd, w=['pp'], key='pp')
        K.dma('pool', obd[:], obdd, w=['obd'], key='obd')
        K.dma('pool', rot[:], rotd, w=['rot'], key='rot')
        K.dma('pool', tc_b[:], tcd, w=['tc_b'], key='tc_b')
        K.dma('pool', tb_b[:], tbd, w=['tb_b'], key='tb_b')
        K.op('dve', lambda e: e.tensor_scalar(out=qgs[:], in0=pp[:, PP_QG:PP_QG + 1], scalar1=0.125, scalar2=None,
                                              op0=ALU.mult), r=['pp'], w=['qgs'])

        NR = 2
        wk_sq = [sb(f"wk_sq{i}", [128, 8, 130], BF16) for i in range(NR)]
        wk_rstd = [sb(f"wk_rstd{i}", [128, 130]) for i in range(NR)]
        hn_sq = [sb(f"hn_sq{i}", [128, 512], BF16) for i in range(NR)]
        hn_rstd = [sb(f"hn_rstd{i}", [128, 512]) for i in range(NR)]
        hn_kn = [sb(f"hn_kn{i}", [128, 512]) for i in range(NR)]
        hn_knb = [sb(f"hn_knb{i}", [128, 512], BF16) for i in range(NR)]
        ring = {'wk': 0, 'hn': 0}

        def load_w(dst, src, nkc, c0, c1, name, r0=0):
            for kc in range(nkc):
                c = c0
                while c < c1:
                    ce = min(c + 2048, c1)
                    K.dma('pool', dst[:, kc, c - c0:ce - c0], src[r0 + kc * 128:r0 + (kc + 1) * 128, c:ce],
                          w=[name], key=name)
                    c = ce

        def rstd_from(src, src_res, n_feat, out_ap, res_out):
            P = src.shape[0]
            K.op('act', lambda e: e.activation(out=out_ap, in_=src, func=AF.Ln, bias=epsb[:P, :],
                                               scale=1.0 / n_feat), r=[src_res, 'epsb'], w=[res_out])
            K.op('act', lambda e: e.activation(out=out_ap, in_=out_ap, func=AF.Exp, scale=-0.5),
                 r=[res_out], w=[res_out])

        def run(g):
            for _ in g:
                pass

        def rmsnorm_fm_g(xt, xname, n, gcol, xn, xnname, banks=(0, 8)):
            k = ring['wk'] % NR
            ring['wk'] += 1
            sq, rs = wk_sq[k], wk_rstd[k]
            sqn, rsn = f'wk_sq{k}', f'wk_rstd{k}'
            K.op('act', lambda e: e.activation(out=sq[:, :, :n], in_=xt[:, :, :n], func=AF.Square),
                 r=[xname], w=[sqn])
            yield
            b = nxt(*banks)
            for kc in range(8):
                K.op('pe', lambda e, kc=kc: e.matmul(ps[b][:, :n], lhsT=onesb[:], rhs=sq[:, kc, :n],
                                                     start=(kc == 0), stop=(kc == 7)),
                     r=[sqn, 'onesb'], w=[psn[b]], inc=(kc == 7))
            yield
            rstd_from(ps[b][:, :n], psn[b], 1024.0, rs[:, :n], rsn)
            yield
            for kc in range(8):
                K.op('dve', lambda e, kc=kc: e.scalar_tensor_tensor(
                    out=xn[:, kc, :n], in0=xt[:, kc, :n], scalar=pp[:, gcol + kc:gcol + kc + 1],
                    in1=rs[:, :n], op0=ALU.mult, op1=ALU.mult),
                     r=[xname, rsn, 'pp'], w=[f'{xnname}.{kc}'])
                if kc % 4 == 3:
                    yield

        def rmsnorm_fm(*a, **kw):
            run(rmsnorm_fm_g(*a, **kw))

        def headnorm_rope_g(src, src_res, n, gain_fn, nseg, cos_ap, sin_ap, cs_res, out_writes, banks=(4, 8)):
            k = ring['hn'] % NR
            ring['hn'] += 1
            sq, rs, kn, knb = hn_sq[k], hn_rstd[k], hn_kn[k], hn_knb[k]
            sqn, rsn, knn, knbn = f'hn_sq{k}', f'hn_rstd{k}', f'hn_kn{k}', f'hn_knb{k}'
            K.op('act', lambda e: e.activation(out=sq[:, :n], in_=src, func=AF.Square), r=[src_res], w=[sqn])
            yield
            b2 = nxt(*banks)
            K.op('pe', lambda e: e.matmul(ps[b2][:, :n], lhsT=obd[:], rhs=sq[:, :n], start=True, stop=True),
                 r=[sqn, 'obd'], w=[psn[b2]])
            yield
            rstd_from(ps[b2][:, :n], psn[b2], 64.0, rs[:, :n], rsn)
            yield
            seg = n // nseg
            for i in range(nseg):
                K.op('dve', lambda e, i=i: e.scalar_tensor_tensor(
                    out=kn[:, i * seg:(i + 1) * seg], in0=src[:, i * seg:(i + 1) * seg], scalar=gain_fn(i),
                    in1=rs[:, i * seg:(i + 1) * seg], op0=ALU.mult, op1=ALU.mult),
                     r=[src_res, rsn, 'pp', 'qgs'], w=[knn])
            yield
            K.op('act', lambda e: e.activation(out=knb[:, :n], in_=kn[:, :n], func=AF.Copy), r=[knn], w=[knbn])
            yield
            b3 = nxt(*banks)
            K.op('pe', lambda e: e.matmul(ps[b3][:, :n], lhsT=rot[:], rhs=knb[:, :n], start=True, stop=True),
                 r=[knbn, 'rot'], w=[psn[b3]])
            yield
            cb = cos_ap.unsqueeze(1).to_broadcast([128, nseg, seg])
            sbb = sin_ap.unsqueeze(1).to_broadcast([128, nseg, seg])

            def v3(ap):
                return ap.rearrange("p (s n) -> p s n", s=nseg)
            K.op('dve', lambda e: e.tensor_tensor(out=v3(rs[:, :n]), in0=v3(ps[b3][:, :n]), in1=sbb, op=ALU.mult),
                 r=[psn[b3], cs_res, rsn], w=[rsn])
            K.op('pool', lambda e: e.tensor_tensor(out=v3(kn[:, :n]), in0=v3(kn[:, :n]), in1=cb, op=ALU.mult),
                 r=[knn, cs_res], w=[knn])
            yield
            K.op('dve', lambda e: e.tensor_tensor(out=kn[:, :n], in0=kn[:, :n], in1=rs[:, :n], op=ALU.add),
                 r=[knn, rsn], w=[knn])
            yield
            out_writes(kn, knn)
            yield

        def headnorm_rope(*a, **kw):
            run(headnorm_rope_g(*a, **kw))

        with ExitStack() as esA:
            sbA = mk(esA)
            ksA = [sbA("ksA0", [128, SEQ], BF16), sbA("ksA1", [128, SEQ], BF16)]
            kwT = sbA("kwT", [128, SEQ], BF16)
            VsA = sbA("VsA", [128, NT, 2, 65], BF16)
            VwA = sbA("VwA", [128, NT, 2, 65], BF16)
            kcmpT = sbA("kcmpT", [128, 512], BF16)
            vcA = sbA("vcA", [128, 4, 2, 193], BF16)
            K.op('pool', lambda e: e.memset(VsA[:, :, :, 64:65], 1.0), w=['VsA'])
            K.op('pool', lambda e: e.memset(VwA[:, :, :, 64:65], 1.0), w=['VwA'])
            K.op('pool', lambda e: e.memset(vcA[:, :, :, 64:65], 1.0), w=['vcA'])
            for c4 in range(4):
                cs_ = slice(c4 * 2048, (c4 + 1) * 2048)
                K.dma('pool', ksA[0][64:128, cs_], indd[:, cs_], w=['ksA0'], key='ind0')
                K.dma('pool', ksA[1][0:64, cs_], indd[:, cs_], w=['ksA1'], key='ind1')
            for g in range(2):
                K.dma('pool', vcA[:, :, g, 65:193], cmd, w=['vcA'], key='cm')

            if stop == 'p0':
                K.barrier()
                dump('pp', pp[:], 'pp'); dump('identb', identb[:], 'identb'); dump('rot', rot[:], 'rot')
                dump('ind', ksA[0][64:128, 0:4096], 'ksA0'); dump('vcA', vcA[:].rearrange("p a b c -> p (a b c)"), 'vcA')
                stop_here()
            with ExitStack() as es1:
                sb1 = mk(es1)
                kvT = [sb1("kcT", [128, 16, 513], BF16), sb1("vcT", [128, 16, 513], BF16)]
                kvn = ['kcT', 'vcT']
                for i in range(2):
                    K.op('pool', lambda e, i=i: e.memset(kvT[i][:, :, 512:513], 0.0), w=[kvn[i]])
                W1 = [sb1("W1k", [128, 32, 256], BF16), sb1("W1v", [128, 32, 256], BF16)]
                W2 = [sb1("W2k", [128, 2, 64], BF16), sb1("W2v", [128, 2, 64], BF16)]
                peb = [sb1("pekb", [128, 32], BF16), sb1("pevb", [128, 32], BF16)]
                ccs = sb1("ccs", [128, 2, 512]);
                K.dma('sp', ccs[:, 0, :], cosC, w=['ccs'], key='ccs')
                K.dma('sp', ccs[:, 1, :], sinC, w=['ccs'], key='ccs')
                for i in range(2):
                    for q4 in range(4):
                        K.dma('pool', W1[i][:, q4 * 8:(q4 + 1) * 8, :].rearrange("p a b -> p (a b)"),
                              w1d[i][:, q4 * 2048:(q4 + 1) * 2048], w=[f'W1{i}'], key=f'W1{i}')
                    K.dma('pool', W2[i][:].rearrange("p a b -> p (a b)"), w2d[i], w=[f'W2{i}'], key=f'W2{i}')
                    K.dma('pool', peb[i][:], ped[i], w=[f'peb{i}'], key=f'peb{i}')
                es1a = ExitStack()
                sb1a = mk(es1a)
                wkv = sb1a("wkv", [128, 8, 768], BF16)
                load_w(wkv, w_in, 8, 512, 1280, 'wkv')
                xt2 = [sb1a(f"xt{i}", [128, 8, 128]) for i in range(2)]
                cs2 = [sb1a(f"cs{i}", [128, 2, 128]) for i in range(2)]
                xn1s = [sb1a(f"xn1{i}", [128, 8, 128], BF16) for i in range(2)]

                def issue_loads(t):
                    s = t % 2
                    K.dma('sp', xt2[s][:], xts[t], w=[f'xt{s}'], key=f'xt{s}')
                    K.dma('sp', cs2[s][:, 0, :], cosT[:, t * 128:(t + 1) * 128], w=[f'cs{s}'], key=f'cs{s}')
                    K.dma('sp', cs2[s][:, 1, :], sinT[:, t * 128:(t + 1) * 128], w=[f'cs{s}'], key=f'cs{s}')

                bKs = {}

                def tileA_g(t):
                    s = t % 2
                    xn1 = xn1s[s]; xn1n = f'xn1{s}'
                    yield from rmsnorm_fm_g(xt2[s], f'xt{s}', 128, PP_GMIX, xn1, xn1n, (0, 4))
                    bA = nxt(0, 4)
                    for i in range(2):
                        for kc in range(8):
                            K.op('pe', lambda e, i=i, kc=kc: e.matmul(
                                ps[bA][:, i * 128:(i + 1) * 128], lhsT=wkv[:, kc, i * 128:(i + 1) * 128],
                                rhs=xn1[:, kc, :], start=(kc == 0), stop=(kc == 7)),
                                 r=['wkv', f'{xn1n}.{kc}'], w=[psn[bA]], inc=(kc == 7))
                        yield
                    for kc in range(8):
                        K.op('pe', lambda e, kc=kc: e.matmul(ps[bA][:, 256:512], lhsT=xn1[:, kc, :],
                                                             rhs=wkv[:, kc, 512:768], start=(kc == 0), stop=(kc == 7)),
                             r=['wkv', f'{xn1n}.{kc}'], w=[psn[bA]], inc=(kc == 7))
                    yield
                    bK = nxt(0, 4)
                    bKs[t] = bK
                    for i in range(2):
                        for kc in range(8):
                            K.op('pe', lambda e, i=i, kc=kc: e.matmul(
                                ps[bK][:, i * 128:(i + 1) * 128], lhsT=wkv[:, kc, (2 + i) * 128:(3 + i) * 128],
                                rhs=xn1[:, kc, :], start=(kc == 0), stop=(kc == 7)),
                                 r=['wkv', f'{xn1n}.{kc}'], w=[psn[bK]], inc=(kc == 7))
                        yield
                    for i in range(2):
                        K.op('act', lambda e, i=i: e.activation(
                            out=kvT[i][:, :, 8 * t:8 * t + 8].rearrange("p ph n -> p n ph"),
                            in_=ps[bA][:, i * 128:(i + 1) * 128].rearrange("p (n ph) -> p n ph", ph=16), func=AF.Copy),
                             r=[psn[bA]], w=[kvn[i]])
                    yield
                    K.op('dve', lambda e: e.tensor_copy(out=VsA[:, t, :, 0:64],
                                                        in_=ps[bA][:, 256:384].rearrange("p (g d) -> p g d", g=2)),
                         r=[psn[bA]], w=['VsA'])
                    K.op('dve', lambda e: e.tensor_copy(out=VwA[:, t, :, 0:64],
                                                        in_=ps[bA][:, 384:512].rearrange("p (g d) -> p g d", g=2)),
                         r=[psn[bA]], w=['VwA'])
                    yield

                def tileB_g(t):
                    s = t % 2
                    bK = bKs.pop(t)

                    def kw_writes(fin, fn_, t=t):
                        K.op('act', lambda e: e.activation(out=ksA[0][0:64, t * 128:(t + 1) * 128],
                                                           in_=fin[0:64, 0:128], func=AF.Copy),
                             r=[fn_], w=['ksA0'])
                        K.op('act', lambda e: e.activation(out=ksA[1][64:128, t * 128:(t + 1) * 128],
                                                           in_=fin[64:128, 0:128], func=AF.Copy),
                             r=[fn_], w=['ksA1'])
                        K.op('act', lambda e: e.activation(out=kwT[:, t * 128:(t + 1) * 128], in_=fin[:, 128:256],
                                                           func=AF.Copy), r=[fn_], w=['kwT'])
                    yield from headnorm_rope_g(ps[bK][:, 0:256], psn[bK], 256, lambda i: pp[:, PP_KG + 1 + i:PP_KG + 2 + i], 2,
                                               cs2[s][:, 0, :], cs2[s][:, 1, :], f'cs{s}', kw_writes, (4, 8))

                NT1 = NT if stop != 'p1t' else 2
                issue_loads(0)
                if NT1 > 1:
                    issue_loads(1)
                run(tileA_g(0))
                for t in range(NT1):
                    gens = [tileB_g(t)]
                    if t + 1 < NT1:
                        gens.append(tileA_g(t + 1))
                    while gens:
                        for g_ in list(gens):
                            try:
                                next(g_)
                            except StopIteration:
                                gens.remove(g_)
                    if t + 2 < NT1:
                        issue_loads(t + 2)
                K.barrier()
                if stop in ('p1', 'p1t'):
                    dump('ksA0', ksA[0][:], 'ksA0'); dump('ksA1', ksA[1][:], 'ksA1'); dump('kwT', kwT[:], 'kwT')
                    dump('VsA', VsA[:].rearrange("p a b c -> p (a b c)"), 'VsA')
                    dump('VwA', VwA[:].rearrange("p a b c -> p (a b c)"), 'VwA')
                    stop_here()
                es1a.close()
                if os.environ.get('KMARK'):
                    print('MARK compress starts after op', K.nops)
                cbias = sb1("cbias", [128, 4])
                hidT = [[sb1(f"hid{i}{g}", [128, 2, 512], BF16) for g in range(2)] for i in range(2)]
                gz = sb1("gz", [128, 512]); gu = sb1("gu", [128, 512]); ge = sb1("ge", [128, 512])
                for i in range(2):
                    for hh in range(2):
                        b = nxt()
                        for l in range(32):
                            K.op('pe', lambda e, l=l: e.matmul(ps[b][:, 0:1], lhsT=W1[i][0:64, l, hh * 128:(hh + 1) * 128],
                                                               rhs=peb[i][0:64, l:l + 1], start=(l == 0), stop=(l == 31)),
                                 r=[f'W1{i}', f'peb{i}'], w=[psn[b]], inc=(l == 31))
                        K.op('act', lambda e: e.activation(out=cbias[:, i * 2 + hh:i * 2 + hh + 1], in_=ps[b][:, 0:1],
                                                           func=AF.Copy), r=[psn[b]], w=['cbias'])
                for i in range(2):
                    for g in range(2):
                        for hh in range(2):
                            b = nxt()
                            for l in range(32):
                                K.op('pe', lambda e, l=l: e.matmul(
                                    ps[b][:, 0:512], lhsT=W1[i][64 * g:64 * g + 64, l, hh * 128:(hh + 1) * 128],
                                    rhs=kvT[i][64 * g:64 * g + 64, l % 16, l // 16:l // 16 + 512], start=(l == 0), stop=(l == 31)),
                                     r=[f'W1{i}', kvn[i]], w=[psn[b]], inc=(l == 31))
                            cb_ap = cbias[:, i * 2 + hh:i * 2 + hh + 1]
                            K.op('dve', lambda e: e.tensor_scalar(out=gz[:], in0=ps[b][:, 0:512], scalar1=cb_ap,
                                                                  scalar2=None, op0=ALU.add),
                                 r=[psn[b], 'cbias'], w=['gz'])
                            K.op('pool', lambda e: e.tensor_tensor(out=gu[:], in0=gz[:], in1=gz[:], op=ALU.mult),
                                 r=['gz'], w=['gu'])
                            K.op('pool', lambda e: e.tensor_scalar(out=gu[:], in0=gu[:], scalar1=0.044715, scalar2=1.0,
                                                                   op0=ALU.mult, op1=ALU.add), r=['gu'], w=['gu'])
                            K.op('pool', lambda e: e.tensor_tensor(out=gu[:], in0=gu[:], in1=gz[:], op=ALU.mult),
                                 r=['gu', 'gz'], w=['gu'])
                            K.op('act', lambda e: e.activation(out=ge[:], in_=gu[:], func=AF.Exp, scale=-1.5957691216),
                                 r=['gu'], w=['ge'])
                            K.op('dve', lambda e: e.tensor_scalar(out=ge[:], in0=ge[:], scalar1=1.0, scalar2=None,
                                                                  op0=ALU.add), r=['ge'], w=['ge'])
                            K.op('dve', lambda e: e.reciprocal(out=ge[:], in_=ge[:]), r=['ge'], w=['ge'])
                            K.op('dve', lambda e: e.tensor_tensor(out=hidT[i][g][:, hh, :], in0=gz[:], in1=ge[:],
                                                                  op=ALU.mult), r=['gz', 'ge'], w=[f'hid{i}{g}'])
                b = nxt(0, 4)
                for g in range(2):
                    for hh in range(2):
                        K.op('pe', lambda e, g=g, hh=hh: e.matmul(ps[b][64 * g:64 * g + 64, 0:512], lhsT=W2[0][:, hh, :],
                                                                   rhs=hidT[0][g][:, hh, :], start=(hh == 0),
                                                                   stop=(hh == 1)),
                             r=['W20', f'hid0{g}'], w=[psn[b]], inc=(hh == 1))

                def kc_writes(fin, fn_):
                    K.op('act', lambda e: e.activation(out=kcmpT[:], in_=fin[:, 0:512], func=AF.Copy),
                         r=[fn_], w=['kcmpT'])
                headnorm_rope(ps[b][:, 0:512], psn[b], 512, lambda i: pp[:, PP_KG:PP_KG + 1], 1,
                              ccs[:, 0, :], ccs[:, 1, :], 'ccs', kc_writes)
                for g in range(2):
                    for c in range(4):
                        b = nxt()
                        for hh in range(2):
                            K.op('pe', lambda e, hh=hh: e.matmul(ps[b][:, 0:64], lhsT=hidT[1][g][:, hh, c * 128:(c + 1) * 128],
                                                                 rhs=W2[1][:, hh, :], start=(hh == 0), stop=(hh == 1)),
                                 r=['W21', f'hid1{g}'], w=[psn[b]], inc=(hh == 1))
                        K.op('act', lambda e: e.activation(out=vcA[:, c, g, 0:64], in_=ps[b][:, 0:64], func=AF.Copy),
                             r=[psn[b]], w=['vcA'])

            K.barrier()
            if stop == 'p1b':
                dump('kcmpT', kcmpT[:], 'kcmpT'); dump('vcA', vcA[:].rearrange("p a b c -> p (a b c)"), 'vcA')
                stop_here()
            with ExitStack() as es2:
                sb2 = mk(es2)
                wq = sb2("wq", [128, 8, 512], BF16); wg = sb2("wg", [128, 8, 24], BF16)
                wc = sb2("wc", [128, 8, 1536], BF16); wo = sb2("wo", [128, 8, 1024], BF16)
                load_w(wq, w_in, 8, 0, 512, 'wq'); load_w(wg, w_in, 8, 1280, 1304, 'wg')
                load_w(wc, w_in, 8, 1304, 2840, 'wc'); load_w(wo, w_o, 8, 0, 1024, 'wo')
                xs2 = [sb2(f"xs{i}", [128, 8, 130]) for i in range(2)]
                csq2 = [sb2(f"csq{i}", [128, 2, 128]) for i in range(2)]
                ibt2 = [sb2(f"ibt{i}", [128, 128]) for i in range(2)]
                cct2 = [sb2(f"cct{i}", [128, 4, 128], BF16) for i in range(2)]
                xn2 = sb2("xn2", [128, 8, 130], BF16)
                ocsq = xn2[:, 0:4, :]
                Qp2 = [sb2(f"Qp{i}", [128, 512], BF16) for i in range(2)]
                QA2 = [[[sb2(f"QA{i}{g}{h}", [128, 512], BF16) for h in range(2)] for g in range(2)] for i in range(2)]
                NET = 3
                ETs = [sb2(f"ET{i}", [128, 512], BF16) for i in range(NET)]
                eti = {'i': 0}
                Gt2 = [sb2(f"Gt{i}", [128, 24]) for i in range(2)]; getmp = sb2("getmp", [128, 24])
                ccsb = sb2("ccsb", [128, 130]); prod = sb2("prod", [128, 130]); cacc = sb2("cacc", [128, 128])
                oconv = sb2("oconv", [128, 4, 128])
                oc_rstd = ccsb
                mixedT2 = [sb2(f"mixedT{i}", [128, 8, 128], BF16) for i in range(2)]
                OC = sb2("OC", [128, 8, 193]); OS = sb2("OS", [128, 8, 65]); OW = sb2("OW", [128, 8, 65])
                rsm = sb2("rsm", [128, 3, 8]); coef = sb2("coef", [128, 3, 8])
                impb = sb2("impb", [128, 128]); m8a = sb2("m8a", [128, 8]); m8b = sb2("m8b", [128, 8])
                mscr = sb2("mscr", [128, 128])
                biasq2 = [sb2(f"biasq{i}", [128, 128]) for i in range(2)]; biasw2 = [sb2(f"biasw{i}", [128, 128]) for i in range(2)]
                oatt = sb2("oatt", [128, 8, 64]); otmp = sb2("otmp", [128, 8, 64])
                o_n = otmp[:].rearrange("p a b -> p (a b)")
                ssq = sb2("ssq", [128, 1]); ssl = sb2("ssl", [128, 1]); ssr = sb2("ssr", [128, 1])
                HQ = 2
                xs_h = sb2("xs_h", [128, 8, HQ + 2]); csq_h = sb2("csq_h", [128, 2, HQ])
                ibt_h = sb2("ibt_h", [128, 128]); cct_h = sb2("cct_h", [128, 4, HQ], BF16)
                Qp_h = sb2("Qp_h", [128, 4 * HQ], BF16)
                QA_h = [[sb2(f"QAh{g}{h}", [128, 4 * HQ], BF16) for h in range(2)] for g in range(2)]
                Gt_h = sb2("Gt_h", [128, 24]); mixedT_h = sb2("mixedT_h", [128, 8, HQ], BF16)

                def bufset(k):
                    T, q_lo, nq, j, main = slots[k]
                    if main:
                        p = j % 2
                        return (xs2[p], csq2[p], ibt2[p], cct2[p], Qp2[p], QA2[p], Gt2[p], mixedT2[p], str(p))
                    return (xs_h, csq_h, ibt_h, cct_h, Qp_h, QA_h, Gt_h, mixedT_h, 'h')
                BG = (6, 8)
                slots = []
                for j in range(NSLOT):
                    slots.append((4 * j + 2, 128 - HQ, HQ, j, False))
                    slots.append((4 * j + 3, 0, 128, j, True))
                NSL = len(slots)
                bgq = []

                pmul = {'n': 1}

                def pump1():
                    while bgq:
                        try:
                            next(bgq[0][1])
                            return
                        except StopIteration:
                            bgq.pop(0)

                def pump():
                    for _ in range(pmul['n']):
                        pump1()

                def ensure(tag):
                    while any(t == tag for t, _ in bgq):
                        pump1()

                def next_et():
                    i = eti['i'] % NET
                    eti['i'] += 1
                    return ETs[i], f'ET{i}'

                def v4(ap):
                    return ap.rearrange("p (r n) -> p r n", r=4)

                def pre_g(k):
                    T, q_lo, nq, j, main = slots[k]
                    xs, csq, ibt, cct, Qp, QA, Gt, mixedT, sfx = bufset(k)
                    ncol = nq + 2
                    n4 = 4 * nq
                    ti = 2 * j + (1 if main else 0)
                    xsn = f'xs{sfx}'
                    if q_lo >= 2:
                        K.dma('sp', xs[:, :, 0:ncol], xts[T][:, :, q_lo - 2:q_lo + nq], w=[xsn], key=xsn)
                    else:
                        K.dma('sp', xs[:, :, 0:2], xts[T - 1][:, :, 126:128], w=[xsn], key=xsn)
                        K.dma('sp', xs[:, :, 2:130], xts[T], w=[xsn], key=xsn)
                    t0 = T * 128 + q_lo
                    csn = f'csq{sfx}'
                    K.dma('sp', csq[:, 0, :nq], cosT[:, t0:t0 + nq], w=[csn], key=csn)
                    K.dma('sp', csq[:, 1, :nq], sinT[:, t0:t0 + nq], w=[csn], key=csn)
                    ibn = f'ibt{sfx}'
                    K.dma('sp', ibt[:nq, :], ibd[ti][q_lo:q_lo + nq, :], w=[ibn], key=ibn)
                    ncmp = T // 16 + 1
                    ccn = f'cct{sfx}'
                    for c in range(ncmp):
                        K.dma('pool', cct[:, c, :nq], ccd[ti, c][:, q_lo:q_lo + nq], w=[ccn], key=ccn)
                    yield
                    yield from rmsnorm_fm_g(xs, xsn, ncol, PP_GMIX, xn2, 'xn2', BG)
                    Qpn = f'Qp{sfx}'; QAn = f'QA{sfx}'
                    bQ = nxt(*BG)
                    for r in range(4):
                        for kc in range(8):
                            K.op('pe', lambda e, r=r, kc=kc: e.matmul(
                                ps[bQ][:, r * nq:(r + 1) * nq], lhsT=wq[:, kc, r * 128:(r + 1) * 128],
                                rhs=xn2[:, kc, 2:ncol], start=(kc == 0), stop=(kc == 7)),
                                 r=['wq', f'xn2.{kc}'], w=[psn[bQ]], inc=(kc == 7))
                        yield

                    def q_writes(fin, fn_):
                        K.op('act', lambda e: e.activation(out=Qp[:, :n4], in_=fin[:, :n4], func=AF.Copy),
                             r=[fn_], w=[Qpn])
                        for h in range(2):
                            K.op('pool', lambda e, h=h: e.tensor_copy(out=QA[0][h][0:64, :n4], in_=fin[0:64, :n4]),
                                 r=[fn_], w=[QAn + f'0{h}q'])
                            K.op('act', lambda e, h=h: e.activation(out=QA[1][h][64:128, :n4], in_=fin[64:128, :n4],
                                                                    func=AF.Copy), r=[fn_], w=[QAn + f'1{h}q'])
                    yield from headnorm_rope_g(ps[bQ][:, :n4], psn[bQ], n4, lambda i: qgs[:, 0:1], 4,
                                               csq[:, 0, :nq], csq[:, 1, :nq], csn, q_writes, BG)
                    Gtn = f'Gt{sfx}'
                    bG = nxt(*BG)
                    for kc in range(8):
                        K.op('pe', lambda e, kc=kc: e.matmul(ps[bG][:nq, 0:24], lhsT=xn2[:, kc, 2:ncol], rhs=wg[:, kc, :],
                                                             start=(kc == 0), stop=(kc == 7)),
                             r=['wg', f'xn2.{kc}'], w=[psn[bG]], inc=(kc == 7))
                    yield
                    K.op('act', lambda e: e.activation(out=getmp[:nq, :], in_=ps[bG][:nq, 0:24], func=AF.Exp, scale=-1.0),
                         r=[psn[bG]], w=['getmp'])
                    yield
                    K.op('dve', lambda e: e.tensor_scalar(out=getmp[:nq, :], in0=getmp[:nq, :], scalar1=1.0, scalar2=None,
                                                          op0=ALU.add), r=['getmp'], w=['getmp'])
                    K.op('dve', lambda e: e.reciprocal(out=Gt[:nq, :], in_=getmp[:nq, :]), r=['getmp'], w=[Gtn])
                    yield
                    mxn = f'mixedT{sfx}'
                    for f in range(4):
                        b = nxt(*BG)
                        for i in range(3):
                            for kc in range(8):
                                K.op('pe', lambda e, i=i, kc=kc: e.matmul(
                                    ps[b][:, i * 130:i * 130 + ncol],
                                    lhsT=wc[:, kc, i * 512 + f * 128:i * 512 + (f + 1) * 128],
                                    rhs=xn2[:, kc, :ncol], start=(kc == 0), stop=(kc == 7)),
                                     r=['wc', f'xn2.{kc}'], w=[psn[b]], inc=(kc == 7))
                            yield
                        K.op('act', lambda e: e.activation(out=ccsb[:, :ncol], in_=ps[b][:, 130:130 + ncol], func=AF.Copy),
                             r=[psn[b]], w=['ccsb'])
                        yield
                        K.op('dve', lambda e: e.tensor_tensor(out=prod[:, :ncol], in0=ccsb[:, :ncol],
                                                              in1=ps[b][:, 260:260 + ncol], op=ALU.mult),
                             r=['ccsb', psn[b]], w=['prod'])
                        cw = lambda kk: pp[:, PP_CW + 3 * f + kk:PP_CW + 3 * f + kk + 1]
                        K.op('dve', lambda e: e.tensor_scalar(out=cacc[:, :nq], in0=prod[:, 2:ncol], scalar1=cw(2),
                                                              scalar2=None, op0=ALU.mult), r=['prod', 'pp'], w=['cacc'])
                        yield
                        K.op('dve', lambda e: e.scalar_tensor_tensor(out=cacc[:, :nq], in0=prod[:, 1:ncol - 1], scalar=cw(1),
                                                                     in1=cacc[:, :nq], op0=ALU.mult, op1=ALU.add),
                             r=['prod', 'pp', 'cacc'], w=['cacc'])
                        K.op('dve', lambda e: e.scalar_tensor_tensor(out=cacc[:, :nq], in0=prod[:, 0:nq], scalar=cw(0),
                                                                     in1=cacc[:, :nq], op0=ALU.mult, op1=ALU.add),
                             r=['prod', 'pp', 'cacc'], w=['cacc'])
                        yield
                        K.op('dve', lambda e, f=f: e.tensor_tensor(out=oconv[:, f, :nq], in0=cacc[:, :nq],
                                                                   in1=ps[b][:, 2:ncol], op=ALU.mult),
                             r=['cacc', psn[b]], w=['oconv'])
                        yield
                    K.op('act', lambda e: e.activation(out=ocsq[:, :, :nq], in_=oconv[:, :, :nq], func=AF.Square),
                         r=['oconv'], w=[f'xn2.{c_}' for c_ in range(4)])
                    yield
                    b = nxt(*BG)
                    for f in range(4):
                        K.op('pe', lambda e, f=f: e.matmul(ps[b][:, :nq], lhsT=onesb[:], rhs=ocsq[:, f, :nq],
                                                           start=(f == 0), stop=(f == 3)), r=[f'xn2.{f}', 'onesb'], w=[psn[b]], inc=(f == 3))
                    yield
                    rstd_from(ps[b][:, :nq], psn[b], 512.0, oc_rstd[:, :nq], 'ccsb')
                    yield
                    for f in range(4):
                        K.op('dve', lambda e, f=f: e.scalar_tensor_tensor(
                            out=mixedT[:, 4 + f, :nq], in0=oconv[:, f, :nq], scalar=pp[:, PP_OCG + f:PP_OCG + f + 1],
                            in1=oc_rstd[:, :nq], op0=ALU.mult, op1=ALU.mult),
                             r=['oconv', 'ccsb', 'pp'], w=[mxn + 'c'])
                    yield

                def att(k):
                    T, q_lo, nq, j, main = slots[k]
                    xs, csq, ibt, cct, Qp, QA, Gt, mixedT, sfx = bufset(k)
                    n4 = 4 * nq
                    Qpn = f'Qp{sfx}'; QAn = f'QA{sfx}'; ibn = f'ibt{sfx}'; ccn = f'cct{sfx}'
                    ncmp = T // 16 + 1
                    PD = 2

                    def bc4(ap):
                        return ap.unsqueeze(1).to_broadcast([128, 4, nq])
                    its = [(c, g) for c in range(ncmp) for g in range(2)]
                    pend = {}

                    def cmp_s1(i):
                        c, g = its[i]
                        bS = nxt(4, 6)
                        K.op('pe', lambda e: e.matmul(ps[bS][:, :n4], lhsT=kcmpT[64 * g:64 * g + 64, c * 128:(c + 1) * 128],
                                                      rhs=Qp[64 * g:64 * g + 64, :n4], start=True, stop=False),
                             r=['kcmpT', Qpn], w=[psn[bS]])
                        K.op('pe', lambda e: e.matmul(v4(ps[bS][:, :n4]), lhsT=identb[:], rhs=bc4(cct[:, c, :nq]),
                                                      start=False, stop=True), r=['identb', ccn], w=[psn[bS]])
                        ET, etn = next_et()
                        K.op('act', lambda e: e.activation(out=ET[:, :n4], in_=ps[bS][:, :n4], func=AF.Exp),
                             r=[psn[bS]], w=[etn])
                        pend[i] = (ET, etn)
                        pump()

                    def cmp_s2(i):
                        c, g = its[i]
                        ET, etn = pend.pop(i)
                        for r in range(4):
                            bank = g * 2 + r // 2
                            off = (r % 2) * 193
                            K.op('pe', lambda e, r=r: e.matmul(
                                ps[bank][:nq, off:off + 193], lhsT=ET[:, r * nq:(r + 1) * nq], rhs=vcA[:, c, g, :],
                                start=(c == 0 and r % 2 == 0), stop=(c == ncmp - 1), skip_group_check=True),
                                 r=[etn, 'vcA'], w=[psn[bank]])
                    for i in range(len(its) + PD):
                        if i < len(its):
                            cmp_s1(i)
                        if i - PD >= 0:
                            cmp_s2(i - PD)
                    for bk in range(4):
                        K.op('act', lambda e, bk=bk: e.activation(
                            out=OC[:nq, 2 * bk:2 * bk + 2, :].rearrange("p a b -> p (a b)"), in_=ps[bk][:nq, 0:386],
                            func=AF.Copy), r=[psn[bk]], w=['OC'])
                    pump()

                    def attn(tiles, kind, Oout, oname, g_list=(0, 1)):
                        its2 = [(tau, g) for tau in tiles for g in g_list]
                        pend2 = {}
                        Vt = VsA if kind == 's' else VwA
                        vtn = 'VsA' if kind == 's' else 'VwA'

                        def s1(i):
                            tau, g = its2[i]
                            bS = nxt(2, 6)
                            static = None
                            if tau == T:
                                static = (tc_b, 'tc_b')
                            elif kind == 'w' and tau == T - 4:
                                static = (tb_b, 'tb_b')
                            if kind == 's':
                                h = 0 if tau < 32 else 1
                                K.op('pe', lambda e: e.matmul(ps[bS][:, :n4], lhsT=ksA[g][:, tau * 128:(tau + 1) * 128],
                                                              rhs=QA[g][h][:, :n4], start=True, stop=(static is None)),
                                     r=[f'ksA{g}', QAn + f'{g}{h}q', QAn + f'{g}{h}b'], w=[psn[bS]])
                            else:
                                K.op('pe', lambda e: e.matmul(ps[bS][:, :n4],
                                                              lhsT=kwT[64 * g:64 * g + 64, tau * 128:(tau + 1) * 128],
                                                              rhs=Qp[64 * g:64 * g + 64, :n4], start=True,
                                                              stop=(static is None)),
                                     r=['kwT', Qpn], w=[psn[bS]])
                            if static is not None:
                                K.op('pe', lambda e: e.matmul(v4(ps[bS][:, :n4]), lhsT=identb[:],
                                                              rhs=bc4(static[0][:, q_lo:q_lo + nq]), start=False, stop=True),
                                     r=['identb', static[1]], w=[psn[bS]])
                            ET, etn = next_et()
                            if tau < 3:
                                K.op('act', lambda e: e.activation(out=ET[:, :n4], in_=ps[bS][:, :n4], func=AF.Exp,
                                                                   bias=pp[:, PP_PAD + tau:PP_PAD + tau + 1]),
                                     r=[psn[bS], 'pp'], w=[etn])
                            else:
                                K.op('act', lambda e: e.activation(out=ET[:, :n4], in_=ps[bS][:, :n4], func=AF.Exp),
                                     r=[psn[bS]], w=[etn])
                            pend2[i] = (ET, etn)
                            pump()

                        def s2(i):
                            tau, g = its2[i]
                            ET, etn = pend2.pop(i)
                            K.op('pe', lambda e: e.matmul(ps[g][:65, :n4], lhsT=Vt[:, tau, g, :], rhs=ET[:, :n4],
                                                          start=(tau == tiles[0]), stop=(tau == tiles[-1])),
                                 r=[etn, vtn], w=[psn[g]])
                        for i in range(len(its2) + PD):
                            if i < len(its2):
                                s1(i)
                            if i - PD >= 0:
                                s2(i - PD)
                        OT = oatt[:].rearrange("p a b -> p (a b)")
                        for g in g_list:
                            K.op('pool', lambda e: e.memset(OT[64:66, :n4], 0.0), w=['oatt'])
                            K.op('act', lambda e, g=g: e.activation(out=OT[:65, :n4], in_=ps[g][:65, :n4], func=AF.Copy),
                                 r=[psn[g]], w=['oatt'])
                            bT = nxt(2, 6)
                            for r in range(4):
                                K.op('pe', lambda e, r=r: e.transpose(out=ps[bT][:nq, r * 66:(r + 1) * 66],
                                                                      in_=OT[:66, r * nq:(r + 1) * nq],
                                                                      identity=identf[:66, :66]),
                                     r=['oatt', 'identf'], w=[psn[bT]])
                            K.op('act', lambda e, g=g: e.activation(
                                out=Oout[:nq, 4 * g:4 * g + 4, :],
                                in_=ps[bT][:nq, 0:264].rearrange("p (a b) -> p a b", a=4)[:, :, 0:65],
                                func=AF.Copy), r=[psn[bT]], w=[oname])
                            pump()

                    K.op('dve', lambda e: e.tensor_scalar(out=rsm[:nq, 0, :], in0=OC[:nq, :, 64], scalar1=1e-30, scalar2=None,
                                                          op0=ALU.max), r=['OC'], w=['rsm'])
                    K.op('dve', lambda e: e.reciprocal(out=rsm[:nq, 0, :], in_=rsm[:nq, 0, :]), r=['rsm'], w=['rsm'])
                    for g in range(2):
                        h0 = 4 * g
                        biasq = biasq2[g]; biasw = biasw2[g]; bqn = f'biasq{g}'; bwn = f'biasw{g}'
                        K.op('dve', lambda e: e.tensor_scalar(out=impb[:nq, :], in0=OC[:nq, h0, 65:193],
                                                              scalar1=rsm[:nq, 0, h0:h0 + 1], scalar2=None, op0=ALU.mult),
                             r=['OC', 'rsm'], w=['impb'])
                        for r in range(1, 4):
                            K.op('dve', lambda e, r=r: e.scalar_tensor_tensor(
                                out=impb[:nq, :], in0=OC[:nq, h0 + r, 65:193], scalar=rsm[:nq, 0, h0 + r:h0 + r + 1],
                                in1=impb[:nq, :], op0=ALU.mult, op1=ALU.add), r=['OC', 'rsm', 'impb'], w=['impb'])
                        K.op('dve', lambda e: e.tensor_tensor(out=impb[:nq, :], in0=impb[:nq, :], in1=ibt[:nq, :], op=ALU.add),
                             r=['impb', ibn], w=['impb'])
                        pump()
                        K.op('dve', lambda e: e.max(out=m8a[:nq, :], in_=impb[:nq, :]), r=['impb'], w=['m8a'])
                        K.op('dve', lambda e: e.match_replace(out=mscr[:nq, :], in_to_replace=m8a[:nq, :],
                                                              in_values=impb[:nq, :], imm_value=-9e9),
                             r=['impb', 'm8a'], w=['mscr'])
                        K.op('dve', lambda e: e.max(out=m8b[:nq, :], in_=mscr[:nq, :]), r=['mscr'], w=['m8b'])
                        pump()
                        K.op('dve', lambda e: e.tensor_scalar(out=mscr[:nq, :], in0=impb[:nq, :], scalar1=m8b[:nq, 7:8],
                                                              scalar2=None, op0=ALU.is_ge), r=['impb', 'm8b', 'mscr'], w=['mscr'])
                        K.op('dve', lambda e: e.tensor_scalar(out=biasq[:nq, :], in0=mscr[:nq, :], scalar1=1.0, scalar2=BIG,
                                                              op0=ALU.subtract, op1=ALU.mult), r=['mscr'], w=[bqn])
                        K.op('pool', lambda e: e.tensor_copy(out=biasw[:nq, 0:64], in_=biasq[:nq, 64:128]),
                             r=[bqn], w=[bwn])
                        K.op('pool', lambda e: e.tensor_copy(out=biasw[:nq, 64:128], in_=biasq[:nq, 0:64]),
                             r=[bqn, bwn], w=[bwn])
                        pump()
                    attn(list(range(max(T - 4, 0), T + 1)), 'w', OW, 'OW')
                    for g in range(2):
                        biasq = biasq2[g]; biasw = biasw2[g]; bqn = f'biasq{g}'; bwn = f'biasw{g}'
                        bT = nxt(4, 6)
                        K.op('pe', lambda e: e.transpose(out=ps[bT][:, 0:nq], in_=biasq[:nq, :], identity=identf[:nq, :nq]),
                             r=[bqn, 'identf'], w=[psn[bT]])
                        K.op('pe', lambda e: e.transpose(out=ps[bT][:, 128:128 + nq], in_=biasw[:nq, :],
                                                         identity=identf[:nq, :nq]), r=[bwn, 'identf'], w=[psn[bT]])
                        if g == 0:
                            rows = slice(64, 128); lo_src = ps[bT][64:128, 128:128 + nq]; hi_src = ps[bT][64:128, 0:nq]
                        else:
                            rows = slice(0, 64); lo_src = ps[bT][0:64, 0:nq]; hi_src = ps[bT][0:64, 128:128 + nq]
                        for h, src in ((0, lo_src), (1, hi_src)):
                            K.op('dve', lambda e, h=h, src=src: e.tensor_copy(
                                out=v4(QA[g][h][rows, :n4]), in_=src.unsqueeze(1).to_broadcast([64, 4, nq])),
                                 r=[psn[bT]], w=[QAn + f'{g}{h}b'])
                        pump()
                    attn(list(range(T + 1)), 's', OS, 'OS')

                def post_a(k):
                    T, q_lo, nq, j, main = slots[k]
                    xs, csq, ibt, cct, Qp, QA, Gt, mixedT, sfx = bufset(k)
                    Gtn = f'Gt{sfx}'
                    for br, O_, on in ((1, OS, 'OS'), (2, OW, 'OW')):
                        K.op('dve', lambda e, br=br, O_=O_: e.tensor_scalar(out=rsm[:nq, br, :], in0=O_[:nq, :, 64],
                                                                            scalar1=1e-30, scalar2=None, op0=ALU.max),
                             r=[on], w=['rsm'])
                        K.op('dve', lambda e, br=br: e.reciprocal(out=rsm[:nq, br, :], in_=rsm[:nq, br, :]),
                             r=['rsm'], w=['rsm'])
                    K.op('dve', lambda e: e.tensor_tensor(out=coef[:nq, :, :], in0=rsm[:nq, :, :],
                                                          in1=Gt[:nq, :].rearrange("p (h b) -> p b h", b=3), op=ALU.mult),
                         r=['rsm', Gtn], w=['coef'])

                    def cbc(br):
                        return coef[:nq, br, :].unsqueeze(2).to_broadcast([nq, 8, 64])
                    K.op('dve', lambda e: e.tensor_tensor(out=oatt[:nq], in0=OC[:nq, :, 0:64], in1=cbc(0), op=ALU.mult),
                         r=['OC', 'coef'], w=['oatt'])
                    K.op('pool', lambda e: e.tensor_tensor(out=otmp[:nq], in0=OS[:nq, :, 0:64], in1=cbc(1), op=ALU.mult),
                         r=['OS', 'coef'], w=['otmp'])
                    K.op('dve', lambda e: e.tensor_tensor(out=oatt[:nq], in0=oatt[:nq], in1=otmp[:nq], op=ALU.add),
                         r=['oatt', 'otmp'], w=['oatt'])
                    K.op('pool', lambda e: e.tensor_tensor(out=otmp[:nq], in0=OW[:nq, :, 0:64], in1=cbc(2), op=ALU.mult),
                         r=['OW', 'coef', 'oatt'], w=['otmp'])
                    K.op('dve', lambda e: e.tensor_tensor(out=oatt[:nq], in0=oatt[:nq], in1=otmp[:nq], op=ALU.add),
                         r=['oatt', 'otmp'], w=['oatt'])
                    oflat = oatt[:nq].rearrange("p a b -> p (a b)")
                    K.op('act', lambda e: e.activation(out=o_n[:nq, :], in_=oflat, func=AF.Square, accum_out=ssq[:nq, :]),
                         r=['oatt'], w=['otmp', 'ssq'])
                    K.op('act', lambda e: e.activation(out=ssl[:nq, :], in_=ssq[:nq, :], func=AF.Ln, bias=epsb[:nq, :],
                                                       scale=1.0 / 512.0), r=['ssq', 'epsb'], w=['ssl'])
                    K.op('act', lambda e: e.activation(out=ssr[:nq, :], in_=ssl[:nq, :], func=AF.Exp, scale=-0.5),
                         r=['ssl'], w=['ssr'])
                    K.op('dve', lambda e: e.tensor_scalar(out=o_n[:nq, :], in0=oflat, scalar1=ssr[:nq, 0:1], scalar2=None,
                                                          op0=ALU.mult), r=['oatt', 'ssr', 'otmp'], w=['otmp'])

                def post_b_g(k):
                    T, q_lo, nq, j, main = slots[k]
                    xs, csq, ibt, cct, Qp, QA, Gt, mixedT, sfx = bufset(k)
                    ncol = nq + 2
                    n4 = 4 * nq
                    xsn = f'xs{sfx}'; mxn = f'mixedT{sfx}'
                    bT = nxt(*BG)
                    for f in range(4):
                        K.op('pe', lambda e, f=f: e.transpose(out=ps[bT][:, f * nq:(f + 1) * nq],
                                                              in_=o_n[:nq, f * 128:(f + 1) * 128], identity=identf[:nq, :nq]),
                             r=['otmp', 'identf'], w=[psn[bT]])
                    yield
                    for f in range(4):
                        K.op('dve', lambda e, f=f: e.tensor_scalar(out=mixedT[:, f, :nq], in0=ps[bT][:, f * nq:(f + 1) * nq],
                                                                   scalar1=pp[:, PP_OAG + f:PP_OAG + f + 1], scalar2=None,
                                                                   op0=ALU.mult), r=[psn[bT], 'pp'], w=[mxn + 'a'])
                        if f % 2 == 1:
                            yield
                    for half in range(2):
                        b = nxt(*BG)
                        for mm in range(4):
                            m = half * 4 + mm
                            for kc in range(8):
                                K.op('pe', lambda e, m=m, mm=mm, kc=kc: e.matmul(
                                    ps[b][:, mm * nq:(mm + 1) * nq], lhsT=wo[:, kc, m * 128:(m + 1) * 128],
                                    rhs=mixedT[:, kc, :nq], start=(kc == 0), stop=(kc == 7)),
                                     r=['wo', mxn + 'a', mxn + 'c'], w=[psn[b]], inc=(kc == 7))
                            yield
                        K.op('dve', lambda e: e.tensor_tensor(out=xs[:, half * 4:half * 4 + 4, 2:ncol], in0=v4(ps[b][:, :n4]),
                                                              in1=xs[:, half * 4:half * 4 + 4, 2:ncol], op=ALU.add),
                             r=[psn[b], xsn], w=[xsn])
                        yield
                    if main:
                        K.dma('sp', hmd[j][:, :, 2:130], xs[:, :, 2:130], r=[xsn], w=['hmd'], key='hmd')
                    yield
                    yield from rmsnorm_fm_g(xs[:, :, 2:ncol], xsn, nq, PP_GFFN, xn2, 'xn2', BG)
                    if main:
                        K.dma('sp', hn3d[j][:, :, 2:130], xn2[:, :, 0:128], r=[f'xn2.{c_}' for c_ in range(8)], w=['hn3d'], key='hn3d')
                    else:
                        K.dma('sp', hn3d[j][:, :, 0:2], xn2[:, :, nq - 2:nq], r=[f'xn2.{c_}' for c_ in range(8)], w=['hn3d'], key='hn3d')
                    yield

                run(pre_g(0))
                run(pre_g(1))
                for j in range(NSLOT):
                    kh, km = 2 * j, 2 * j + 1
                    ensure(('pre', kh))
                    if j >= 1:
                        bgq.append((('postb', km - 2), post_b_g(km - 2)))
                    pmul['n'] = 1
                    att(kh)
                    pmul['n'] = 1
                    if j >= 1:
                        ensure(('postb', km - 2))
                    post_a(kh)
                    ensure(('pre', km))
                    bgq.append((('postb', kh), post_b_g(kh)))
                    if j + 1 < NSLOT:
                        bgq.append((('pre', kh + 2), pre_g(kh + 2)))
                        bgq.append((('pre', km + 2), pre_g(km + 2)))
                    iters_ = 2 * (4 * j + 4) + 2 * min(4 * j + 4, 5) + 4
                    pmul['n'] = max(1, min(3, -(-150 // iters_)))
                    att(km)
                    pmul['n'] = 1
                    ensure(('postb', kh))
                    post_a(km)
                    if stop == 'p2s' and j == int(os.environ.get('KSLOT', '0')):
                        k = km
                        while bgq:
                            pump()
                        run(post_b_g(k))
                        K.barrier()
                        dump('hm', bufset(k)[0][:].rearrange("p a b -> p (a b)"), f'xs{bufset(k)[8]}')
                        dump('oatt', oatt[:].rearrange("p a b -> p (a b)"), 'oatt')
                        stop_here()
                while bgq:
                    pump()
                run(post_b_g(NSL - 1))

        K.barrier()
        with ExitStack() as es3:
            sb3 = mk(es3)
            hacc = sb3("hacc", [128, NSLOT, 8, 128])
            fence = sb3("fence", [128, 1])
            hn3 = sb3("hn3", [128, NSLOT, 8, 130], BF16)
            es3w = ExitStack()
            sb3w = mk(es3w)
            PASSES = [(0, 4), (4, 10), (10, 16), (16, 22)]
            wugs = [sb3w(f"wug{i}", [128, 8, 768], BF16) for i in range(2)]
            wuus = [sb3w(f"wuu{i}", [128, 8, 768], BF16) for i in range(2)]
            wdns = [sb3w(f"wdn{i}", [128, 6, 1024], BF16) for i in range(2)]
            actTs = [sb3w(f"actT{i}", [128, 6, 128], BF16) for i in range(2)]
            NFA = 4
            faccs = [sb3w(f"facc{i}", [128, 128]) for i in range(NFA)]
            fss = [sb3w(f"fs{i}", [128, 128]) for i in range(NFA)]
            fus = [sb3w(f"fu{i}", [128, 128]) for i in range(NFA)]

            def load_pass(p):
                c0, c1 = PASSES[p]
                bi = p % 2
                load_w(wugs[bi], w_up, 8, c0 * 128, c1 * 128, f'wug{bi}')
                load_w(wuus[bi], w_up, 8, DFF + c0 * 128, DFF + c1 * 128, f'wuu{bi}')
                for f in range(c1 - c0):
                    K.dma('pool', wdns[bi][:, f, :], w_down[(c0 + f) * 128:(c0 + f + 1) * 128, :], w=[f'wdn{bi}'],
                          key=f'wdn{bi}')
            load_pass(0)
            for j in range(NSLOT):
                K.dma('sp', hn3[:, j], hn3d[j], r=['hn3d'], w=['hn3'], key='hn3')
                K.dma('sp', hacc[:, j], hmd[j][:, :, 2:130], r=['hmd'], w=['hacc_all'], key='hacc')
            K.op('dve', lambda e: e.memset(fence[:], 0.0), r=['hacc_all'], w=[f'hacc{j}' for j in range(NSLOT)])
            fi = {'i': 0}
            for p in range(len(PASSES)):
                c0, c1 = PASSES[p]
                ncf = c1 - c0
                bi = p % 2
                wug, wuu, wdn = wugs[bi], wuus[bi], wdns[bi]
                if p + 1 < len(PASSES):
                    load_pass(p + 1)

                def down(j, ncf=ncf, wdn=wdn, bi=bi):
                    aT = actTs[j % 2]; an = f'actT{j % 2}'
                    for half in range(2):
                        b = nxt()
                        for mm in range(4):
                            m = half * 4 + mm
                            for f in range(ncf):
                                K.op('pe', lambda e, m=m, mm=mm, f=f: e.matmul(
                                    ps[b][:, mm * 128:(mm + 1) * 128], lhsT=wdn[:, f, m * 128:(m + 1) * 128],
                                    rhs=aT[:, f, :], start=(f == 0), stop=(f == ncf - 1)),
                                     r=[f'wdn{bi}', an], w=[psn[b]], inc=(f == ncf - 1))
                        K.op('dve', lambda e, half=half: e.tensor_tensor(
                            out=hacc[:, j, half * 4:half * 4 + 4, :], in0=ps[b][:].rearrange("p (a n) -> p a n", a=4),
                            in1=hacc[:, j, half * 4:half * 4 + 4, :], op=ALU.add), r=[psn[b], f'hacc{j}'], w=[f'hacc{j}'])
                for j in range(NSLOT):
                    aT = actTs[j % 2]; an = f'actT{j % 2}'
                    for f in range(ncf):
                        F = c0 + f
                        bg = nxt()
                        for kc in range(8):
                            K.op('pe', lambda e, kc=kc: e.matmul(ps[bg][:, 0:130], lhsT=wug[:, kc, f * 128:(f + 1) * 128],
                                                                 rhs=hn3[:, j, kc, :], start=(kc == 0), stop=(kc == 7)),
                                 r=[f'wug{bi}', 'hn3'], w=[psn[bg]], inc=(kc == 7))
                        for kc in range(8):
                            K.op('pe', lambda e, kc=kc: e.matmul(ps[bg][:, 130:260], lhsT=wuu[:, kc, f * 128:(f + 1) * 128],
                                                                 rhs=hn3[:, j, kc, :], start=(kc == 0), stop=(kc == 7)),
                                 r=[f'wuu{bi}', 'hn3'], w=[psn[bg]], inc=(kc == 7))
                        k = fi['i'] % NFA
                        fi['i'] += 1
                        facc = faccs[k]; fs = fss[k]; fu = fus[k]
                        fan = f'facc{k}'; fsn = f'fs{k}'; fun = f'fu{k}'
                        fw = lambda kk: pp[:, PP_FW + 3 * F + kk:PP_FW + 3 * F + kk + 1]
                        K.op('act', lambda e: e.activation(out=facc[:], in_=ps[bg][:, 2:130], func=AF.Identity,
                                                           bias=pp[:, PP_FB + F:PP_FB + F + 1], scale=fw(2)),
                             r=[psn[bg], 'pp'], w=[fan])
                        K.op('act', lambda e: e.activation(out=fu[:], in_=ps[bg][:, 132:260], func=AF.Copy),
                             r=[psn[bg]], w=[fun])
                        K.op('dve', lambda e: e.scalar_tensor_tensor(out=facc[:], in0=ps[bg][:, 1:129], scalar=fw(1),
                                                                     in1=facc[:], op0=ALU.mult, op1=ALU.add),
                             r=[psn[bg], 'pp', fan], w=[fan])
                        K.op('dve', lambda e: e.scalar_tensor_tensor(out=facc[:], in0=ps[bg][:, 0:128], scalar=fw(0),
                                                                     in1=facc[:], op0=ALU.mult, op1=ALU.add),
                             r=[psn[bg], 'pp', fan], w=[fan])
                        K.op('act', lambda e: e.activation(out=fs[:], in_=facc[:], func=AF.Silu), r=[fan], w=[fsn])
                        K.op('dve', lambda e, f=f: e.tensor_tensor(out=aT[:, f, :], in0=fs[:], in1=fu[:], op=ALU.mult),
                             r=[fsn, fun], w=[an])
                        if f == 1 and j > 0:
                            down(j - 1)
                down(NSLOT - 1)
            K.barrier()
            es3w.close()
            wpg = sb3("wpg", [128, 8, 1024], BF16); wpe = sb3("wpe", [128, 2, 1024], BF16)
            hn4s = [sb3(f"hn4{i}", [128, 8, 128], BF16) for i in range(2)]
            pt2 = [sb3(f"pt{i}", [128, 2, 128]) for i in range(2)]
            ptbs = [sb3(f"ptb{i}", [128, 2, 128], BF16) for i in range(2)]
            pe_es = [sb3(f"pe_e{i}", [128, 512]) for i in range(2)]
            pe_ts = [sb3(f"pe_t{i}", [128, 512]) for i in range(2)]
            ob2 = [sb3(f"ob{i}", [128, 8, 128]) for i in range(2)]
            load_w(wpg, w_pg, 8, 0, 1024, 'wpg'); load_w(wpe, w_pe, 2, 0, 1024, 'wpe')

            def pleA_g(j):
                s = j % 2
                K.dma('sp', pt2[s][:], ptd[j], w=[f'pt{s}'], key=f'pt{s}')
                K.op('pool', lambda e: e.tensor_copy(out=ptbs[s][:], in_=pt2[s][:]), r=[f'pt{s}'], w=[f'ptb{s}'])
                yield
                yield from rmsnorm_fm_g(hacc[:, j], f'hacc{j}', 128, PP_GPLE, hn4s[s], f'hn4{s}', (6, 8))

            def pleB_g(j):
                s = j % 2
                ob = ob2[s]; hn4 = hn4s[s]; ptb = ptbs[s]
                for half in range(2):
                    pe_e = pe_es[half]; pe_t = pe_ts[half]; pen = f'pe_e{half}'; ptn = f'pe_t{half}'
                    b1 = nxt(0, 6); b2 = nxt(0, 6)
                    for mm in range(4):
                        m = half * 4 + mm
                        for kc in range(8):
                            K.op('pe', lambda e, m=m, mm=mm, kc=kc: e.matmul(
                                ps[b1][:, mm * 128:(mm + 1) * 128], lhsT=wpg[:, kc, m * 128:(m + 1) * 128],
                                rhs=hn4[:, kc, :], start=(kc == 0), stop=(kc == 7)),
                                 r=['wpg', f'hn4{s}.{kc}'], w=[psn[b1]], inc=(kc == 7))
                        for kc in range(2):
                            K.op('pe', lambda e, m=m, mm=mm, kc=kc: e.matmul(
                                ps[b2][:, mm * 128:(mm + 1) * 128], lhsT=wpe[:, kc, m * 128:(m + 1) * 128],
                                rhs=ptb[:, kc, :], start=(kc == 0), stop=(kc == 1)),
                                 r=['wpe', f'ptb{s}'], w=[psn[b2]], inc=(kc == 1))
                        yield
                    K.op('act', lambda e: e.activation(out=pe_e[:], in_=ps[b1][:], func=AF.Sigmoid),
                         r=[psn[b1]], w=[pen])
                    yield
                    K.op('dve', lambda e: e.tensor_tensor(out=pe_t[:], in0=pe_e[:], in1=ps[b2][:], op=ALU.mult),
                         r=[pen, psn[b2]], w=[ptn])
                    yield
                    K.op('dve', lambda e, half=half: e.tensor_tensor(
                        out=ob[:, half * 4:half * 4 + 4, :], in0=pe_t[:].rearrange("p (a n) -> p a n", a=4),
                        in1=hacc[:, j, half * 4:half * 4 + 4, :], op=ALU.add), r=[ptn, f'hacc{j}'], w=[f'ob{s}'])
                    yield
                K.dma('sp', outT[j], ob[:], r=[f'ob{s}'], w=[f'outd{s}'], key=f'out{s}')
                yield

            run(pleA_g(0))
            for j in range(NSLOT):
                gens = [pleB_g(j)]
                if j + 1 < NSLOT:
                    gens.append(pleA_g(j + 1))
                while gens:
                    for g_ in list(gens):
                        try:
                            next(g_)
                        except StopIteration:
                            gens.remove(g_)
            K.finish('sp', ['out0', 'out1'])
    return nc


_NC_CACHE = {}


def _core_tables(r):
    pad = 3 - r
    half = 8
    inv_freq = (ROPE_THETA ** (-np.arange(half, dtype=np.float32) * 2.0 / 16.0)).astype(np.float32)

    def cs_tables(pos):
        ang = pos.astype(np.float32)[None, :] * inv_freq[:, None]
        c = np.ones((64, pos.shape[0]), np.float32)
        s = np.zeros((64, pos.shape[0]), np.float32)
        c[0:8] = np.cos(ang); c[8:16] = np.cos(ang)
        s[0:8] = np.sin(ang); s[8:16] = np.sin(ang)
        return np.concatenate([c, c], 0), np.concatenate([s, s], 0)
    pos = np.arange(SEQ) - 128 * pad
    cosT, sinT = cs_tables(pos)
    cpos = 16 * (np.arange(512) - 8 * pad) + 31
    cosC, sinC = cs_tables(cpos)
    cc = np.full((2 * NSLOT, 4, 128, 128), -BIG, np.float32)
    ib = np.zeros((2 * NSLOT, 128, 128), np.float32)
    b0 = 2 * pad
    blk = np.arange(128)
    for j in range(NSLOT):
        for main in range(2):
            ti = 2 * j + main
            T = 4 * j + 2 + main
            tq = 128 * T + np.arange(128)
            for c in range(4):
                n_p = 128 * c + np.arange(128)
                valid = (16 * n_p[:, None] + 31 <= tq[None, :]) & (n_p[:, None] >= 8 * pad)
                cc[ti, c] = np.where(valid, 0.0, -BIG)
            cur = tq // 64
            v = np.zeros((128, 128), np.float32)
            B = blk[None, :]
            C = cur[:, None]
            v = np.where(B > C, -1e9 - B * 1e6, v)
            v = np.where(B == b0, 1e9, v)
            v = np.where(B == C - 1, 2e9, v)
            v = np.where(B == C, 3e9, v)
            v = np.where(B < b0, -3e9 - B * 1e6, v)
            ib[ti] = v
    padb = np.zeros((128, 4), np.float32)
    for t in range(3):
        if t < pad:
            padb[:, t] = -BIG
    return cosT, sinT, cosC, sinC, cc, ib, padb


def _prep(x, p, ln_mix_g, w_in, qn_g, kn_g, pe_k, pe_v, w_ck1, w_ck2, w_cv1, w_cv2,
           conv_w, on_att_g, on_conv_g, w_o, ln_ffn_g, w_up, ffn_conv_w, ffn_conv_b, w_down,
           ln_ple_g, w_pg, w_pe):
    f32 = np.float32
    x = np.asarray(x, f32); p = np.asarray(p, f32)
    w_in0 = np.asarray(w_in, f32)[0]
    qperm = np.array([g * 256 + r * 64 + d for r in range(4) for g in range(2) for d in range(64)])
    cols = np.concatenate([qperm, np.arange(512, 640), np.arange(640, 768), np.arange(768, 896),
                           np.arange(1024, 1152), np.arange(896, 1024), np.arange(1152, 1280),
                           np.arange(1280, 2840)])
    w_in_p = np.ascontiguousarray(w_in0[:, cols])

    def colpack(v, n):
        return np.asarray(v, f32).reshape(n, 128).T
    pp_base = np.zeros((128, NPP), f32)
    pp_base[:, PP_GMIX:PP_GMIX + 8] = colpack(ln_mix_g[0], 8)
    pp_base[:, PP_GFFN:PP_GFFN + 8] = colpack(ln_ffn_g[0], 8)
    pp_base[:, PP_GPLE:PP_GPLE + 8] = colpack(ln_ple_g[0], 8)
    pp_base[:, PP_OAG:PP_OAG + 4] = colpack(on_att_g[0], 4)
    pp_base[:, PP_OCG:PP_OCG + 4] = colpack(on_conv_g[0], 4)
    pp_base[:, PP_QG] = np.tile(np.asarray(qn_g, f32)[0], 2)
    for b in range(3):
        pp_base[:, PP_KG + b] = np.tile(np.asarray(kn_g, f32)[0, b], 2)
    cw = np.asarray(conv_w, f32)[0]
    for f in range(4):
        for k in range(3):
            pp_base[:, PP_CW + 3 * f + k] = cw[k, f * 128:(f + 1) * 128]
    fw = np.asarray(ffn_conv_w, f32)[0]
    fb = np.asarray(ffn_conv_b, f32)[0]
    for F in range(NF):
        for k in range(3):
            pp_base[:, PP_FW + 3 * F + k] = fw[k, F * 128:(F + 1) * 128]
        pp_base[:, PP_FB + F] = fb[F * 128:(F + 1) * 128]

    def w1pack(w):
        a = np.asarray(w, f32)[0].transpose(1, 0, 2).reshape(64, 32 * 256)
        return np.ascontiguousarray(np.concatenate([a, a], 0))

    def w2pack(w):
        a = np.asarray(w, f32)[0].reshape(2, 128, 64).transpose(1, 0, 2).reshape(128, 128)
        return np.ascontiguousarray(a)

    def pepack(pe):
        a = np.asarray(pe, f32)[0].T
        return np.ascontiguousarray(np.concatenate([a, a], 0))
    kk = np.arange(128)
    tcd = np.where(kk[:, None] <= kk[None, :], 0.0, -BIG).astype(f32)
    tbd = np.where(kk[:, None] > kk[None, :], 0.0, -BIG).astype(f32)
    keys = np.arange(SEQ)
    indd = (((keys[None, :] // 64) % 64) == np.arange(64)[:, None]).astype(f32)
    n_p = np.arange(512)
    sblk = np.arange(128)
    ov = np.clip(np.minimum(16 * n_p[:, None] + 32, 64 * sblk[None, :] + 64)
                 - np.maximum(16 * n_p[:, None], 64 * sblk[None, :]), 0, None).astype(f32) / 32.0
    cmd = np.ascontiguousarray(ov.reshape(4, 128, 128).transpose(1, 0, 2))
    rotd = np.zeros((128, 128), f32)
    obdd = np.zeros((128, 128), f32)
    for g in range(2):
        o = 64 * g
        obdd[o:o + 64, o:o + 64] = 1.0
        for m in range(8):
            rotd[o + m + 8, o + m] = -1.0
            rotd[o + m, o + m + 8] = 1.0
    shared = dict(w_in=w_in_p, w_o=np.asarray(w_o, f32)[0], w_up=np.asarray(w_up, f32)[0],
                  w_down=np.asarray(w_down, f32)[0], w_pg=np.asarray(w_pg, f32)[0], w_pe=np.asarray(w_pe, f32)[0],
                  w1k=w1pack(w_ck1), w1v=w1pack(w_cv1), w2k=w2pack(w_ck2), w2v=w2pack(w_cv2),
                  pek=pepack(pe_k), pev=pepack(pe_v), tcd=tcd, tbd=tbd, indd=indd, cmd=cmd, rotd=rotd, obdd=obdd)
    tabs = [_core_tables(r) for r in range(4)]
    in_maps = []
    for c in range(8):
        b, r = c // 4, c % 4
        pad = 3 - r
        xprog = np.zeros((SEQ, D), f32)
        xprog[128 * pad:] = x[b, :SEQ - 128 * pad]
        xts = np.ascontiguousarray(xprog.reshape(NT, 128, 8, 128).transpose(0, 3, 2, 1))
        pb = p[0, b].reshape(NT, 128, 2, 128)
        ptd = np.ascontiguousarray(pb[r::4].transpose(0, 3, 2, 1))
        cosT, sinT, cosC, sinC, cc, ib, padb = tabs[r]
        ppc = pp_base.copy()
        ppc[:, PP_PAD:PP_PAD + 4] = padb
        m = dict(shared)
        m.update(xts=xts, ptd=ptd, cosT=cosT, sinT=sinT, cosC=cosC, sinC=sinC, ccd=cc, ibd=ib, ppd=ppc)
        in_maps.append(m)
    return in_maps


def kernel(**inputs):
    f32 = np.float32
    in_maps = _prep(**inputs)
    if 'nc' not in _NC_CACHE:
        _NC_CACHE['nc'] = build_nc()
    res = run_bass_kernel_spmd(_NC_CACHE['nc'], in_maps, core_ids=list(range(8)))
    out = np.zeros((2, SEQ, D), f32)
    for c in range(8):
        b, r = c // 4, c % 4
        o = np.asarray(res.results[c]["outT"], f32)
        o = o.transpose(0, 3, 2, 1).reshape(NSLOT, 128, D)
        out[b].reshape(NT, 128, D)[r::4] = o
    return out
```

```python
from contextlib import ExitStack
import os
import numpy as np
import concourse.bass as bass
import concourse.mybir as mybir
from concourse.bass_utils import run_bass_kernel_spmd

F32 = mybir.dt.float32
BF16 = mybir.dt.bfloat16
AF = mybir.ActivationFunctionType
ALU = mybir.AluOpType
AX = mybir.AxisListType

D = 1024
SEQ = 8192
NT = 64
NSLOT = 16
DFF = 2816
NF = 22
EPS = 1e-6
BIG = 30000.0
ROPE_THETA = 500000.0

PP_GMIX = 0
PP_GFFN = 8
PP_GPLE = 16
PP_OAG = 24
PP_OCG = 28
PP_QG = 32
PP_KG = 33
PP_CW = 36
PP_FW = 48
PP_FB = 114
PP_PAD = 136
NPP = 140


class _Stop(Exception):
    pass


class Trk:
    B = 2000

    def __init__(s, nc, es):
        s.nc, s.es = nc, es
        s.E = {'pe': nc.tensor, 'act': nc.scalar, 'dve': nc.vector, 'pool': nc.gpsimd, 'sp': nc.sync}
        s.seq = {e: 0 for e in s.E}
        s.esems = {e: [] for e in s.E}
        s.know = {}
        s.gi = {}
        s.gcount = 0
        s.clock = {}
        s.lastw = {}
        s.readers = {}
        s.dsem = {}
        s.dcnt = {}
        s.nsem = 0

    def _newsem(s, name):
        s.nsem += 1
        return s.es.enter_context(s.nc.semaphore(name))

    def _esem(s, e, seq):
        k = (seq - 1) // s.B
        while len(s.esems[e]) <= k:
            s.esems[e].append(s._newsem(f"s_{e}{len(s.esems[e])}"))
        return s.esems[e][k], (seq - 1) % s.B + 1

    def _known(s, me):
        return s.know.setdefault(me, {})

    def _merge(s, me, clock):
        kn = s._known(me)
        for d, v in clock.items():
            if v > kn.get(d, 0):
                kn[d] = v

    def _sync(s, me, r, w):
        deps = []
        for x in r:
            lw = s.lastw.get(x)
            if lw is not None:
                deps.append(lw)
            if x.startswith('ps'):
                for rd in s.readers.get(x, ()):
                    if rd[0] == 'eng' and rd[1] != me:
                        deps.append(rd)
        for x in w:
            lw = s.lastw.get(x)
            if lw is not None:
                deps.append(lw)
            deps.extend(s.readers.get(x, ()))
        kn = s._known(me)
        need = {}
        for d in deps:
            if d[0] == 'eng':
                _, e, q = d
                if e == me and me == 'pe':
                    continue
                dim = e
            else:
                _, k, q = d
                dim = 'dma:' + k
            if q > kn.get(dim, 0):
                need[dim] = max(need.get(dim, 0), q)
        waits = []
        for dim, q in sorted(need.items(), key=lambda t: -s.gi.get(t, 0)):
            if q <= kn.get(dim, 0):
                continue
            if dim.startswith('dma:'):
                sem, v = s.dsem[dim[4:]], q
            else:
                sem, v = s._esem(dim, q)
            waits.append((sem, v))
            kn[dim] = q
            s._merge(me, s.clock.get((dim, q), {}))
        return waits

    def op(s, me, fn, r=(), w=(), inc=True):
        if not inc:
            for sem, v in s._sync(me, r, w):
                s.E[me].wait_ge(sem, v)
            ins = fn(s.E[me])
            tag = ('eng', me, s.seq[me] + 1)
            for x in r:
                s.readers.setdefault(x, []).append(tag)
            for x in w:
                s.lastw[x] = tag
                s.readers[x] = []
            return ins
        s.nops = getattr(s, 'nops', 0) + 1
        lim = int(os.environ.get('KLIMIT', '0'))
        if lim and s.nops > lim:
            s.barrier()
            raise _Stop()
        waits = s._sync(me, r, w)
        eng = s.E[me]
        attach = None
        if waits and me != 'pe':
            attach = waits.pop()
        for sem, v in waits:
            eng.wait_ge(sem, v)
        ins = fn(eng)
        if attach is not None:
            ins._wait_ge(attach[0], attach[1])
        s.seq[me] += 1
        q = s.seq[me]
        sem, _ = s._esem(me, q)
        ins.then_inc(sem, 1)
        ck = dict(s._known(me))
        ck[me] = q
        s.clock[(me, q)] = ck
        s.gcount += 1
        s.gi[(me, q)] = s.gcount
        tag = ('eng', me, q)
        for x in r:
            s.readers.setdefault(x, []).append(tag)
        for x in w:
            s.lastw[x] = tag
            s.readers[x] = []
        return ins

    def dma(s, q, out, in_, r=(), w=(), key=None):
        waits = s._sync(q, r, w)
        for sem, v in waits:
            s.E[q].wait_ge(sem, v)
        if key not in s.dsem:
            s.dsem[key] = s._newsem("d_" + key)
            s.dcnt[key] = 0
        ins = s.E[q].dma_start(out=out, in_=in_)
        ins.then_inc(s.dsem[key], 16)
        s.dcnt[key] += 16
        ck = dict(s._known(q))
        ck['dma:' + key] = s.dcnt[key]
        s.clock[('dma:' + key, s.dcnt[key])] = ck
        s.gcount += 1
        s.gi[('dma:' + key, s.dcnt[key])] = s.gcount
        tag = ('dma', key, s.dcnt[key])
        for x in r:
            s.readers.setdefault(x, []).append(tag)
        for x in w:
            s.lastw[x] = tag
            s.readers[x] = []
        return ins

    def barrier(s):
        for me, eng in s.E.items():
            kn = s._known(me)
            for e in s.E:
                if e != me and s.seq[e] > kn.get(e, 0):
                    sem, v = s._esem(e, s.seq[e])
                    eng.wait_ge(sem, v)
                    kn[e] = s.seq[e]
            for k, cnt in s.dcnt.items():
                if cnt > kn.get('dma:' + k, 0):
                    eng.wait_ge(s.dsem[k], cnt)
                    kn['dma:' + k] = cnt

    def finish(s, q, keys):
        for k in keys:
            s.E[q].wait_ge(s.dsem[k], s.dcnt[k])


def build_nc(stop=None):
    nc = bass.Bass("TRN2", target_bir_lowering=False)
    try:
        _build(nc, stop)
    except _Stop:
        pass
    return nc


def _build(nc, stop):

    def din(name, shape, dt=F32):
        return nc.dram_tensor(name, list(shape), dt, kind="ExternalInput").ap()

    xts = din("xts", [NT, 128, 8, 128])
    ptd = din("ptd", [NSLOT, 128, 2, 128])
    cosT = din("cosT", [128, SEQ]); sinT = din("sinT", [128, SEQ])
    cosC = din("cosC", [128, 512]); sinC = din("sinC", [128, 512])
    w_in = din("w_in", [D, 2840])
    w_o = din("w_o", [D, D]); w_up = din("w_up", [D, 2 * DFF]); w_down = din("w_down", [DFF, D])
    w_pg = din("w_pg", [D, D]); w_pe = din("w_pe", [256, D])
    w1d = [din("w1k", [128, 32 * 256]), din("w1v", [128, 32 * 256])]
    w2d = [din("w2k", [128, 2 * 64]), din("w2v", [128, 2 * 64])]
    ped = [din("pek", [128, 32]), din("pev", [128, 32])]
    ppd = din("ppd", [128, NPP])
    tcd = din("tcd", [128, 128]); tbd = din("tbd", [128, 128])
    ccd = din("ccd", [2 * NSLOT, 4, 128, 128])
    ibd = din("ibd", [2 * NSLOT, 128, 128])
    indd = din("indd", [64, SEQ])
    cmd = din("cmd", [128, 4, 128])
    rotd = din("rotd", [128, 128]); obdd = din("obdd", [128, 128])
    outT = nc.dram_tensor("outT", [NSLOT, 128, 8, 128], F32, kind="ExternalOutput").ap()
    hmd = nc.dram_tensor("hmd", [NSLOT, 128, 8, 130], F32, kind="Internal").ap()
    hn3d = nc.dram_tensor("hn3d", [NSLOT, 128, 8, 130], BF16, kind="Internal").ap()

    with ExitStack() as es:
        K = Trk(nc, es)

        def mk(stack):
            def sb(name, shape, dt=F32):
                return stack.enter_context(nc.sbuf_tensor(name, list(shape), dt))
            return sb
        sb = mk(es)

        ps = [es.enter_context(nc.psum_tensor(f"ps{i}", [128, 512], F32)) for i in range(8)]
        psn = [f"ps{i}" for i in range(8)]
        rr = {}

        def nxt(lo=0, hi=8):
            n = hi - lo
            i = rr.get((lo, hi), 0)
            rr[(lo, hi)] = i + 1
            return lo + i % n

        dkeys = []

        def dump(name, ap2d, res):
            P, n = ap2d.shape[0], ap2d.shape[1]
            d = nc.dram_tensor("dbg_" + name, [P, n], F32, kind="ExternalOutput").ap()
            c = 0
            while c < n:
                ce = min(c + 2048, n)
                K.dma('pool', d[:, c:ce], ap2d[:, c:ce], r=[res], w=['dbg_' + name], key='dbg_' + name)
                c = ce
            dkeys.append('dbg_' + name)

        def stop_here():
            K.finish('pool', dkeys)
            raise _Stop()

        identf = sb("identf", [128, 128]); identb = sb("identb", [128, 128], BF16)
        onesb = sb("onesb", [128, 128], BF16); obd = sb("obd", [128, 128], BF16)
        rot = sb("rot", [128, 128], BF16)
        pp = sb("pp", [128, NPP]); qgs = sb("qgs", [128, 1]); epsb = sb("epsb", [128, 1])
        tc_b = sb("tc_b", [128, 128], BF16); tb_b = sb("tb_b", [128, 128], BF16)
        K.op('pool', lambda e: e.memset(identf[:], 0.0), w=['identf'])
        K.op('pool', lambda e: e.affine_select(out=identf[:], in_=identf[:], compare_op=ALU.not_equal, fill=1.0,
                                               base=0, pattern=[[-1, 128]], channel_multiplier=1),
             r=['identf'], w=['identf'])
        K.op('pool', lambda e: e.tensor_copy(out=identb[:], in_=identf[:]), r=['identf'], w=['identb'])
        K.op('pool', lambda e: e.memset(onesb[:], 1.0), w=['onesb'])
        K.op('pool', lambda e: e.memset(epsb[:], EPS), w=['epsb'])
        K.dma('sp', pp[:], ppd, w=['pp'], key='pp')
        K.dma('pool', obd[:], obdd, w=['obd'], key='obd')
        K.dma('pool', rot[:], rotd, w=['rot'], key='rot')
        K.dma('pool', tc_b[:], tcd, w=['tc_b'], key='tc_b')
        K.dma('pool', tb_b[:], tbd, w=['tb_b'], key='tb_b')
        K.op('dve', lambda e: e.tensor_scalar(out=qgs[:], in0=pp[:, PP_QG:PP_QG + 1], scalar1=0.125, scalar2=None,
                                              op0=ALU.mult), r=['pp'], w=['qgs'])

        NR = 2
        wk_sq = [sb(f"wk_sq{i}", [128, 8, 130], BF16) for i in range(NR)]
        wk_rstd = [sb(f"wk_rstd{i}", [128, 130]) for i in range(NR)]
        hn_sq = [sb(f"hn_sq{i}", [128, 512], BF16) for i in range(NR)]
        hn_rstd = [sb(f"hn_rstd{i}", [128, 512]) for i in range(NR)]
        hn_kn = [sb(f"hn_kn{i}", [128, 512]) for i in range(NR)]
        hn_knb = [sb(f"hn_knb{i}", [128, 512], BF16) for i in range(NR)]
        ring = {'wk': 0, 'hn': 0}

        def load_w(dst, src, nkc, c0, c1, name, r0=0):
            for kc in range(nkc):
                c = c0
                while c < c1:
                    ce = min(c + 2048, c1)
                    K.dma('pool', dst[:, kc, c - c0:ce - c0], src[r0 + kc * 128:r0 + (kc + 1) * 128, c:ce],
                          w=[name], key=name)
                    c = ce

        def rstd_from(src, src_res, n_feat, out_ap, res_out):
            P = src.shape[0]
            K.op('act', lambda e: e.activation(out=out_ap, in_=src, func=AF.Ln, bias=epsb[:P, :],
                                               scale=1.0 / n_feat), r=[src_res, 'epsb'], w=[res_out])
            K.op('act', lambda e: e.activation(out=out_ap, in_=out_ap, func=AF.Exp, scale=-0.5),
                 r=[res_out], w=[res_out])

        def run(g):
            for _ in g:
                pass

        def rmsnorm_fm_g(xt, xname, n, gcol, xn, xnname, banks=(0, 8)):
            k = ring['wk'] % NR
            ring['wk'] += 1
            sq, rs = wk_sq[k], wk_rstd[k]
            sqn, rsn = f'wk_sq{k}', f'wk_rstd{k}'
            K.op('act', lambda e: e.activation(out=sq[:, :, :n], in_=xt[:, :, :n], func=AF.Square),
                 r=[xname], w=[sqn])
            yield
            b = nxt(*banks)
            for kc in range(8):
                K.op('pe', lambda e, kc=kc: e.matmul(ps[b][:, :n], lhsT=onesb[:], rhs=sq[:, kc, :n],
                                                     start=(kc == 0), stop=(kc == 7)),
                     r=[sqn, 'onesb'], w=[psn[b]], inc=(kc == 7))
            yield
            rstd_from(ps[b][:, :n], psn[b], 1024.0, rs[:, :n], rsn)
            yield
            for kc in range(8):
                K.op('dve', lambda e, kc=kc: e.scalar_tensor_tensor(
                    out=xn[:, kc, :n], in0=xt[:, kc, :n], scalar=pp[:, gcol + kc:gcol + kc + 1],
                    in1=rs[:, :n], op0=ALU.mult, op1=ALU.mult),
                     r=[xname, rsn, 'pp'], w=[f'{xnname}.{kc}'])
                if kc % 4 == 3:
                    yield

        def rmsnorm_fm(*a, **kw):
            run(rmsnorm_fm_g(*a, **kw))

        def headnorm_rope_g(src, src_res, n, gain_fn, nseg, cos_ap, sin_ap, cs_res, out_writes, banks=(4, 8)):
            k = ring['hn'] % NR
            ring['hn'] += 1
            sq, rs, kn, knb = hn_sq[k], hn_rstd[k], hn_kn[k], hn_knb[k]
            sqn, rsn, knn, knbn = f'hn_sq{k}', f'hn_rstd{k}', f'hn_kn{k}', f'hn_knb{k}'
            K.op('act', lambda e: e.activation(out=sq[:, :n], in_=src, func=AF.Square), r=[src_res], w=[sqn])
            yield
            b2 = nxt(*banks)
            K.op('pe', lambda e: e.matmul(ps[b2][:, :n], lhsT=obd[:], rhs=sq[:, :n], start=True, stop=True),
                 r=[sqn, 'obd'], w=[psn[b2]])
            yield
            rstd_from(ps[b2][:, :n], psn[b2], 64.0, rs[:, :n], rsn)
            yield
            seg = n // nseg
            for i in range(nseg):
                K.op('dve', lambda e, i=i: e.scalar_tensor_tensor(
                    out=kn[:, i * seg:(i + 1) * seg], in0=src[:, i * seg:(i + 1) * seg], scalar=gain_fn(i),
                    in1=rs[:, i * seg:(i + 1) * seg], op0=ALU.mult, op1=ALU.mult),
                     r=[src_res, rsn, 'pp', 'qgs'], w=[knn])
            yield
            K.op('act', lambda e: e.activation(out=knb[:, :n], in_=kn[:, :n], func=AF.Copy), r=[knn], w=[knbn])
            yield
            b3 = nxt(*banks)
            K.op('pe', lambda e: e.matmul(ps[b3][:, :n], lhsT=rot[:], rhs=knb[:, :n], start=True, stop=True),
                 r=[knbn, 'rot'], w=[psn[b3]])
            yield
            cb = cos_ap.unsqueeze(1).to_broadcast([128, nseg, seg])
            sbb = sin_ap.unsqueeze(1).to_broadcast([128, nseg, seg])

            def v3(ap):
                return ap.rearrange("p (s n) -> p s n", s=nseg)
            K.op('dve', lambda e: e.tensor_tensor(out=v3(rs[:, :n]), in0=v3(ps[b3][:, :n]), in1=sbb, op=ALU.mult),
                 r=[psn[b3], cs_res, rsn], w=[rsn])
            K.op('pool', lambda e: e.tensor_tensor(out=v3(kn[:, :n]), in0=v3(kn[:, :n]), in1=cb, op=ALU.mult),
                 r=[knn, cs_res], w=[knn])
            yield
            K.op('dve', lambda e: e.tensor_tensor(out=kn[:, :n], in0=kn[:, :n], in1=rs[:, :n], op=ALU.add),
                 r=[knn, rsn], w=[knn])
            yield
            out_writes(kn, knn)
            yield

        def headnorm_rope(*a, **kw):
            run(headnorm_rope_g(*a, **kw))

        with ExitStack() as esA:
            sbA = mk(esA)
            ksA = [sbA("ksA0", [128, SEQ], BF16), sbA("ksA1", [128, SEQ], BF16)]
            kwT = sbA("kwT", [128, SEQ], BF16)
            VsA = sbA("VsA", [128, NT, 2, 65], BF16)
            VwA = sbA("VwA", [128, NT, 2, 65], BF16)
            kcmpT = sbA("kcmpT", [128, 512], BF16)
            vcA = sbA("vcA", [128, 4, 2, 193], BF16)
            K.op('pool', lambda e: e.memset(VsA[:, :, :, 64:65], 1.0), w=['VsA'])
            K.op('pool', lambda e: e.memset(VwA[:, :, :, 64:65], 1.0), w=['VwA'])
            K.op('pool', lambda e: e.memset(vcA[:, :, :, 64:65], 1.0), w=['vcA'])
            for c4 in range(4):
                cs_ = slice(c4 * 2048, (c4 + 1) * 2048)
                K.dma('pool', ksA[0][64:128, cs_], indd[:, cs_], w=['ksA0'], key='ind0')
                K.dma('pool', ksA[1][0:64, cs_], indd[:, cs_], w=['ksA1'], key='ind1')
            for g in range(2):
                K.dma('pool', vcA[:, :, g, 65:193], cmd, w=['vcA'], key='cm')

            if stop == 'p0':
                K.barrier()
                dump('pp', pp[:], 'pp'); dump('identb', identb[:], 'identb'); dump('rot', rot[:], 'rot')
                dump('ind', ksA[0][64:128, 0:4096], 'ksA0'); dump('vcA', vcA[:].rearrange("p a b c -> p (a b c)"), 'vcA')
                stop_here()
            with ExitStack() as es1:
                sb1 = mk(es1)
                kvT = [sb1("kcT", [128, 16, 513], BF16), sb1("vcT", [128, 16, 513], BF16)]
                kvn = ['kcT', 'vcT']
                for i in range(2):
                    K.op('pool', lambda e, i=i: e.memset(kvT[i][:, :, 512:513], 0.0), w=[kvn[i]])
                W1 = [sb1("W1k", [128, 32, 256], BF16), sb1("W1v", [128, 32, 256], BF16)]
                W2 = [sb1("W2k", [128, 2, 64], BF16), sb1("W2v", [128, 2, 64], BF16)]
                peb = [sb1("pekb", [128, 32], BF16), sb1("pevb", [128, 32], BF16)]
                ccs = sb1("ccs", [128, 2, 512]);
                K.dma('sp', ccs[:, 0, :], cosC, w=['ccs'], key='ccs')
                K.dma('sp', ccs[:, 1, :], sinC, w=['ccs'], key='ccs')
                for i in range(2):
                    for q4 in range(4):
                        K.dma('pool', W1[i][:, q4 * 8:(q4 + 1) * 8, :].rearrange("p a b -> p (a b)"),
                              w1d[i][:, q4 * 2048:(q4 + 1) * 2048], w=[f'W1{i}'], key=f'W1{i}')
                    K.dma('pool', W2[i][:].rearrange("p a b -> p (a b)"), w2d[i], w=[f'W2{i}'], key=f'W2{i}')
                    K.dma('pool', peb[i][:], ped[i], w=[f'peb{i}'], key=f'peb{i}')
                es1a = ExitStack()
                sb1a = mk(es1a)
                wkv = sb1a("wkv", [128, 8, 768], BF16)
                load_w(wkv, w_in, 8, 512, 1280, 'wkv')
                xt2 = [sb1a(f"xt{i}", [128, 8, 128]) for i in range(2)]
                cs2 = [sb1a(f"cs{i}", [128, 2, 128]) for i in range(2)]
                xn1s = [sb1a(f"xn1{i}", [128, 8, 128], BF16) for i in range(2)]

                def issue_loads(t):
                    s = t % 2
                    K.dma('sp', xt2[s][:], xts[t], w=[f'xt{s}'], key=f'xt{s}')
                    K.dma('sp', cs2[s][:, 0, :], cosT[:, t * 128:(t + 1) * 128], w=[f'cs{s}'], key=f'cs{s}')
                    K.dma('sp', cs2[s][:, 1, :], sinT[:, t * 128:(t + 1) * 128], w=[f'cs{s}'], key=f'cs{s}')

                bKs = {}

                def tileA_g(t):
                    s = t % 2
                    xn1 = xn1s[s]; xn1n = f'xn1{s}'
                    yield from rmsnorm_fm_g(xt2[s], f'xt{s}', 128, PP_GMIX, xn1, xn1n, (0, 4))
                    bA = nxt(0, 4)
                    for i in range(2):
                        for kc in range(8):
                            K.op('pe', lambda e, i=i, kc=kc: e.matmul(
                                ps[bA][:, i * 128:(i + 1) * 128], lhsT=wkv[:, kc, i * 128:(i + 1) * 128],
                                rhs=xn1[:, kc, :], start=(kc == 0), stop=(kc == 7)),
                                 r=['wkv', f'{xn1n}.{kc}'], w=[psn[bA]], inc=(kc == 7))
                        yield
                    for kc in range(8):
                        K.op('pe', lambda e, kc=kc: e.matmul(ps[bA][:, 256:512], lhsT=xn1[:, kc, :],
                                                             rhs=wkv[:, kc, 512:768], start=(kc == 0), stop=(kc == 7)),
                             r=['wkv', f'{xn1n}.{kc}'], w=[psn[bA]], inc=(kc == 7))
                    yield
                    bK = nxt(0, 4)
                    bKs[t] = bK
                    for i in range(2):
                        for kc in range(8):
                            K.op('pe', lambda e, i=i, kc=kc: e.matmul(
                                ps[bK][:, i * 128:(i + 1) * 128], lhsT=wkv[:, kc, (2 + i) * 128:(3 + i) * 128],
                                rhs=xn1[:, kc, :], start=(kc == 0), stop=(kc == 7)),
                                 r=['wkv', f'{xn1n}.{kc}'], w=[psn[bK]], inc=(kc == 7))
                        yield
                    for i in range(2):
                        K.op('act', lambda e, i=i: e.activation(
                            out=kvT[i][:, :, 8 * t:8 * t + 8].rearrange("p ph n -> p n ph"),
                            in_=ps[bA][:, i * 128:(i + 1) * 128].rearrange("p (n ph) -> p n ph", ph=16), func=AF.Copy),
                             r=[psn[bA]], w=[kvn[i]])
                    yield
                    K.op('dve', lambda e: e.tensor_copy(out=VsA[:, t, :, 0:64],
                                                        in_=ps[bA][:, 256:384].rearrange("p (g d) -> p g d", g=2)),
                         r=[psn[bA]], w=['VsA'])
                    K.op('dve', lambda e: e.tensor_copy(out=VwA[:, t, :, 0:64],
                                                        in_=ps[bA][:, 384:512].rearrange("p (g d) -> p g d", g=2)),
                         r=[psn[bA]], w=['VwA'])
                    yield

                def tileB_g(t):
                    s = t % 2
                    bK = bKs.pop(t)

                    def kw_writes(fin, fn_, t=t):
                        K.op('act', lambda e: e.activation(out=ksA[0][0:64, t * 128:(t + 1) * 128],
                                                           in_=fin[0:64, 0:128], func=AF.Copy),
                             r=[fn_], w=['ksA0'])
                        K.op('act', lambda e: e.activation(out=ksA[1][64:128, t * 128:(t + 1) * 128],
                                                           in_=fin[64:128, 0:128], func=AF.Copy),
                             r=[fn_], w=['ksA1'])
                        K.op('act', lambda e: e.activation(out=kwT[:, t * 128:(t + 1) * 128], in_=fin[:, 128:256],
                                                           func=AF.Copy), r=[fn_], w=['kwT'])
                    yield from headnorm_rope_g(ps[bK][:, 0:256], psn[bK], 256, lambda i: pp[:, PP_KG + 1 + i:PP_KG + 2 + i], 2,
                                               cs2[s][:, 0, :], cs2[s][:, 1, :], f'cs{s}', kw_writes, (4, 8))

                NT1 = NT if stop != 'p1t' else 2
                issue_loads(0)
                if NT1 > 1:
                    issue_loads(1)
                run(tileA_g(0))
                for t in range(NT1):
                    gens = [tileB_g(t)]
                    if t + 1 < NT1:
                        gens.append(tileA_g(t + 1))
                    while gens:
                        for g_ in list(gens):
                            try:
                                next(g_)
                            except StopIteration:
                                gens.remove(g_)
                    if t + 2 < NT1:
                        issue_loads(t + 2)
                K.barrier()
                if stop in ('p1', 'p1t'):
                    dump('ksA0', ksA[0][:], 'ksA0'); dump('ksA1', ksA[1][:], 'ksA1'); dump('kwT', kwT[:], 'kwT')
                    dump('VsA', VsA[:].rearrange("p a b c -> p (a b c)"), 'VsA')
                    dump('VwA', VwA[:].rearrange("p a b c -> p (a b c)"), 'VwA')
                    stop_here()
                es1a.close()
                if os.environ.get('KMARK'):
                    print('MARK compress starts after op', K.nops)
                cbias = sb1("cbias", [128, 4])
                hidT = [[sb1(f"hid{i}{g}", [128, 2, 512], BF16) for g in range(2)] for i in range(2)]
                gz = sb1("gz", [128, 512]); gu = sb1("gu", [128, 512]); ge = sb1("ge", [128, 512])
                for i in range(2):
                    for hh in range(2):
                        b = nxt()
                        for l in range(32):
                            K.op('pe', lambda e, l=l: e.matmul(ps[b][:, 0:1], lhsT=W1[i][0:64, l, hh * 128:(hh + 1) * 128],
                                                               rhs=peb[i][0:64, l:l + 1], start=(l == 0), stop=(l == 31)),
                                 r=[f'W1{i}', f'peb{i}'], w=[psn[b]], inc=(l == 31))
                        K.op('act', lambda e: e.activation(out=cbias[:, i * 2 + hh:i * 2 + hh + 1], in_=ps[b][:, 0:1],
                                                           func=AF.Copy), r=[psn[b]], w=['cbias'])
                for i in range(2):
                    for g in range(2):
                        for hh in range(2):
                            b = nxt()
                            for l in range(32):
                                K.op('pe', lambda e, l=l: e.matmul(
                                    ps[b][:, 0:512], lhsT=W1[i][64 * g:64 * g + 64, l, hh * 128:(hh + 1) * 128],
                                    rhs=kvT[i][64 * g:64 * g + 64, l % 16, l // 16:l // 16 + 512], start=(l == 0), stop=(l == 31)),
                                     r=[f'W1{i}', kvn[i]], w=[psn[b]], inc=(l == 31))
                            cb_ap = cbias[:, i * 2 + hh:i * 2 + hh + 1]
                            K.op('dve', lambda e: e.tensor_scalar(out=gz[:], in0=ps[b][:, 0:512], scalar1=cb_ap,
                                                                  scalar2=None, op0=ALU.add),
                                 r=[psn[b], 'cbias'], w=['gz'])
                            K.op('pool', lambda e: e.tensor_tensor(out=gu[:], in0=gz[:], in1=gz[:], op=ALU.mult),
                                 r=['gz'], w=['gu'])
                            K.op('pool', lambda e: e.tensor_scalar(out=gu[:], in0=gu[:], scalar1=0.044715, scalar2=1.0,
                                                                   op0=ALU.mult, op1=ALU.add), r=['gu'], w=['gu'])
                            K.op('pool', lambda e: e.tensor_tensor(out=gu[:], in0=gu[:], in1=gz[:], op=ALU.mult),
                                 r=['gu', 'gz'], w=['gu'])
                            K.op('act', lambda e: e.activation(out=ge[:], in_=gu[:], func=AF.Exp, scale=-1.5957691216),
                                 r=['gu'], w=['ge'])
                            K.op('dve', lambda e: e.tensor_scalar(out=ge[:], in0=ge[:], scalar1=1.0, scalar2=None,
                                                                  op0=ALU.add), r=['ge'], w=['ge'])
                            K.op('dve', lambda e: e.reciprocal(out=ge[:], in_=ge[:]), r=['ge'], w=['ge'])
                            K.op('dve', lambda e: e.tensor_tensor(out=hidT[i][g][:, hh, :], in0=gz[:], in1=ge[:],
                                                                  op=ALU.mult), r=['gz', 'ge'], w=[f'hid{i}{g}'])
                b = nxt(0, 4)
                for g in range(2):
                    for hh in range(2):
                        K.op('pe', lambda e, g=g, hh=hh: e.matmul(ps[b][64 * g:64 * g + 64, 0:512], lhsT=W2[0][:, hh, :],
                                                                   rhs=hidT[0][g][:, hh, :], start=(hh == 0),
                                                                   stop=(hh == 1)),
                             r=['W20', f'hid0{g}'], w=[psn[b]], inc=(hh == 1))

                def kc_writes(fin, fn_):
                    K.op('act', lambda e: e.activation(out=kcmpT[:], in_=fin[:, 0:512], func=AF.Copy),
                         r=[fn_], w=['kcmpT'])
                headnorm_rope(ps[b][:, 0:512], psn[b], 512, lambda i: pp[:, PP_KG:PP_KG + 1], 1,
                              ccs[:, 0, :], ccs[:, 1, :], 'ccs', kc_writes)
                for g in range(2):
                    for c in range(4):
                        b = nxt()
                        for hh in range(2):
                            K.op('pe', lambda e, hh=hh: e.matmul(ps[b][:, 0:64], lhsT=hidT[1][g][:, hh, c * 128:(c + 1) * 128],
                                                                 rhs=W2[1][:, hh, :], start=(hh == 0), stop=(hh == 1)),
                                 r=['W21', f'hid1{g}'], w=[psn[b]], inc=(hh == 1))
                        K.op('act', lambda e: e.activation(out=vcA[:, c, g, 0:64], in_=ps[b][:, 0:64], func=AF.Copy),
                             r=[psn[b]], w=['vcA'])

            K.barrier()
            if stop == 'p1b':
                dump('kcmpT', kcmpT[:], 'kcmpT'); dump('vcA', vcA[:].rearrange("p a b c -> p (a b c)"), 'vcA')
                stop_here()
            with ExitStack() as es2:
                sb2 = mk(es2)
                wq = sb2("wq", [128, 8, 512], BF16); wg = sb2("wg", [128, 8, 24], BF16)
                wc = sb2("wc", [128, 8, 1536], BF16); wo = sb2("wo", [128, 8, 1024], BF16)
                load_w(wq, w_in, 8, 0, 512, 'wq'); load_w(wg, w_in, 8, 1280, 1304, 'wg')
                load_w(wc, w_in, 8, 1304, 2840, 'wc'); load_w(wo, w_o, 8, 0, 1024, 'wo')
                xs2 = [sb2(f"xs{i}", [128, 8, 130]) for i in range(2)]
                csq2 = [sb2(f"csq{i}", [128, 2, 128]) for i in range(2)]
                ibt2 = [sb2(f"ibt{i}", [128, 128]) for i in range(2)]
                cct2 = [sb2(f"cct{i}", [128, 4, 128], BF16) for i in range(2)]
                xn2 = sb2("xn2", [128, 8, 130], BF16)
                ocsq = xn2[:, 0:4, :]
                Qp2 = [sb2(f"Qp{i}", [128, 512], BF16) for i in range(2)]
                QA2 = [[[sb2(f"QA{i}{g}{h}", [128, 512], BF16) for h in range(2)] for g in range(2)] for i in range(2)]
                NET = 3
                ETs = [sb2(f"ET{i}", [128, 512], BF16) for i in range(NET)]
                eti = {'i': 0}
                Gt2 = [sb2(f"Gt{i}", [128, 24]) for i in range(2)]; getmp = sb2("getmp", [128, 24])
                ccsb = sb2("ccsb", [128, 130]); prod = sb2("prod", [128, 130]); cacc = sb2("cacc", [128, 128])
                oconv = sb2("oconv", [128, 4, 128])
                oc_rstd = ccsb
                mixedT2 = [sb2(f"mixedT{i}", [128, 8, 128], BF16) for i in range(2)]
                OC = sb2("OC", [128, 8, 193]); OS = sb2("OS", [128, 8, 65]); OW = sb2("OW", [128, 8, 65])
                rsm = sb2("rsm", [128, 3, 8]); coef = sb2("coef", [128, 3, 8])
                impb = sb2("impb", [128, 128]); m8a = sb2("m8a", [128, 8]); m8b = sb2("m8b", [128, 8])
                mscr = sb2("mscr", [128, 128])
                biasq2 = [sb2(f"biasq{i}", [128, 128]) for i in range(2)]; biasw2 = [sb2(f"biasw{i}", [128, 128]) for i in range(2)]
                oatt = sb2("oatt", [128, 8, 64]); otmp = sb2("otmp", [128, 8, 64])
                o_n = otmp[:].rearrange("p a b -> p (a b)")
                ssq = sb2("ssq", [128, 1]); ssl = sb2("ssl", [128, 1]); ssr = sb2("ssr", [128, 1])
                HQ = 2
                xs_h = sb2("xs_h", [128, 8, HQ + 2]); csq_h = sb2("csq_h", [128, 2, HQ])
                ibt_h = sb2("ibt_h", [128, 128]); cct_h = sb2("cct_h", [128, 4, HQ], BF16)
                Qp_h = sb2("Qp_h", [128, 4 * HQ], BF16)
                QA_h = [[sb2(f"QAh{g}{h}", [128, 4 * HQ], BF16) for h in range(2)] for g in range(2)]
                Gt_h = sb2("Gt_h", [128, 24]); mixedT_h = sb2("mixedT_h", [128, 8, HQ], BF16)

                def bufset(k):
                    T, q_lo, nq, j, main = slots[k]
                    if main:
                        p = j % 2
                        return (xs2[p], csq2[p], ibt2[p], cct2[p], Qp2[p], QA2[p], Gt2[p], mixedT2[p], str(p))
                    return (xs_h, csq_h, ibt_h, cct_h, Qp_h, QA_h, Gt_h, mixedT_h, 'h')
                BG = (6, 8)
                slots = []
                for j in range(NSLOT):
                    slots.append((4 * j + 2, 128 - HQ, HQ, j, False))
                    slots.append((4 * j + 3, 0, 128, j, True))
                NSL = len(slots)
                bgq = []

                pmul = {'n': 1}

                def pump1():
                    while bgq:
                        try:
                            next(bgq[0][1])
                            return
                        except StopIteration:
                            bgq.pop(0)

                def pump():
                    for _ in range(pmul['n']):
                        pump1()

                def ensure(tag):
                    while any(t == tag for t, _ in bgq):
                        pump1()

                def next_et():
                    i = eti['i'] % NET
                    eti['i'] += 1
                    return ETs[i], f'ET{i}'

                def v4(ap):
                    return ap.rearrange("p (r n) -> p r n", r=4)

                def pre_g(k):
                    T, q_lo, nq, j, main = slots[k]
                    xs, csq, ibt, cct, Qp, QA, Gt, mixedT, sfx = bufset(k)
                    ncol = nq + 2
                    n4 = 4 * nq
                    ti = 2 * j + (1 if main else 0)
                    xsn = f'xs{sfx}'
                    if q_lo >= 2:
                        K.dma('sp', xs[:, :, 0:ncol], xts[T][:, :, q_lo - 2:q_lo + nq], w=[xsn], key=xsn)
                    else:
                        K.dma('sp', xs[:, :, 0:2], xts[T - 1][:, :, 126:128], w=[xsn], key=xsn)
                        K.dma('sp', xs[:, :, 2:130], xts[T], w=[xsn], key=xsn)
                    t0 = T * 128 + q_lo
                    csn = f'csq{sfx}'
                    K.dma('sp', csq[:, 0, :nq], cosT[:, t0:t0 + nq], w=[csn], key=csn)
                    K.dma('sp', csq[:, 1, :nq], sinT[:, t0:t0 + nq], w=[csn], key=csn)
                    ibn = f'ibt{sfx}'
                    K.dma('sp', ibt[:nq, :], ibd[ti][q_lo:q_lo + nq, :], w=[ibn], key=ibn)
                    ncmp = T // 16 + 1
                    ccn = f'cct{sfx}'
                    for c in range(ncmp):
                        K.dma('pool', cct[:, c, :nq], ccd[ti, c][:, q_lo:q_lo + nq], w=[ccn], key=ccn)
                    yield
                    yield from rmsnorm_fm_g(xs, xsn, ncol, PP_GMIX, xn2, 'xn2', BG)
                    Qpn = f'Qp{sfx}'; QAn = f'QA{sfx}'
                    bQ = nxt(*BG)
                    for r in range(4):
                        for kc in range(8):
                            K.op('pe', lambda e, r=r, kc=kc: e.matmul(
                                ps[bQ][:, r * nq:(r + 1) * nq], lhsT=wq[:, kc, r * 128:(r + 1) * 128],
                                rhs=xn2[:, kc, 2:ncol], start=(kc == 0), stop=(kc == 7)),
                                 r=['wq', f'xn2.{kc}'], w=[psn[bQ]], inc=(kc == 7))
                        yield

                    def q_writes(fin, fn_):
                        K.op('act', lambda e: e.activation(out=Qp[:, :n4], in_=fin[:, :n4], func=AF.Copy),
                             r=[fn_], w=[Qpn])
                        for h in range(2):
                            K.op('pool', lambda e, h=h: e.tensor_copy(out=QA[0][h][0:64, :n4], in_=fin[0:64, :n4]),
                                 r=[fn_], w=[QAn + f'0{h}q'])
                            K.op('act', lambda e, h=h: e.activation(out=QA[1][h][64:128, :n4], in_=fin[64:128, :n4],
                                                                    func=AF.Copy), r=[fn_], w=[QAn + f'1{h}q'])
                    yield from headnorm_rope_g(ps[bQ][:, :n4], psn[bQ], n4, lambda i: qgs[:, 0:1], 4,
                                               csq[:, 0, :nq], csq[:, 1, :nq], csn, q_writes, BG)
                    Gtn = f'Gt{sfx}'
                    bG = nxt(*BG)
                    for kc in range(8):
                        K.op('pe', lambda e, kc=kc: e.matmul(ps[bG][:nq, 0:24], lhsT=xn2[:, kc, 2:ncol], rhs=wg[:, kc, :],
                                                             start=(kc == 0), stop=(kc == 7)),
                             r=['wg', f'xn2.{kc}'], w=[psn[bG]], inc=(kc == 7))
                    yield
                    K.op('act', lambda e: e.activation(out=getmp[:nq, :], in_=ps[bG][:nq, 0:24], func=AF.Exp, scale=-1.0),
                         r=[psn[bG]], w=['getmp'])
                    yield
                    K.op('dve', lambda e: e.tensor_scalar(out=getmp[:nq, :], in0=getmp[:nq, :], scalar1=1.0, scalar2=None,
                                                          op0=ALU.add), r=['getmp'], w=['getmp'])
                    K.op('dve', lambda e: e.reciprocal(out=Gt[:nq, :], in_=getmp[:nq, :]), r=['getmp'], w=[Gtn])
                    yield
                    mxn = f'mixedT{sfx}'
                    for f in range(4):
                        b = nxt(*BG)
                        for i in range(3):
                            for kc in range(8):
                                K.op('pe', lambda e, i=i, kc=kc: e.matmul(
                                    ps[b][:, i * 130:i * 130 + ncol],
                                    lhsT=wc[:, kc, i * 512 + f * 128:i * 512 + (f + 1) * 128],
                                    rhs=xn2[:, kc, :ncol], start=(kc == 0), stop=(kc == 7)),
                                     r=['wc', f'xn2.{kc}'], w=[psn[b]], inc=(kc == 7))
                            yield
                        K.op('act', lambda e: e.activation(out=ccsb[:, :ncol], in_=ps[b][:, 130:130 + ncol], func=AF.Copy),
                             r=[psn[b]], w=['ccsb'])
                        yield
                        K.op('dve', lambda e: e.tensor_tensor(out=prod[:, :ncol], in0=ccsb[:, :ncol],
                                                              in1=ps[b][:, 260:260 + ncol], op=ALU.mult),
                             r=['ccsb', psn[b]], w=['prod'])
                        cw = lambda kk: pp[:, PP_CW + 3 * f + kk:PP_CW + 3 * f + kk + 1]
                        K.op('dve', lambda e: e.tensor_scalar(out=cacc[:, :nq], in0=prod[:, 2:ncol], scalar1=cw(2),
                                                              scalar2=None, op0=ALU.mult), r=['prod', 'pp'], w=['cacc'])
                        yield
                        K.op('dve', lambda e: e.scalar_tensor_tensor(out=cacc[:, :nq], in0=prod[:, 1:ncol - 1], scalar=cw(1),
                                                                     in1=cacc[:, :nq], op0=ALU.mult, op1=ALU.add),
                             r=['prod', 'pp', 'cacc'], w=['cacc'])
                        K.op('dve', lambda e: e.scalar_tensor_tensor(out=cacc[:, :nq], in0=prod[:, 0:nq], scalar=cw(0),
                                                                     in1=cacc[:, :nq], op0=ALU.mult, op1=ALU.add),
                             r=['prod', 'pp', 'cacc'], w=['cacc'])
                        yield
                        K.op('dve', lambda e, f=f: e.tensor_tensor(out=oconv[:, f, :nq], in0=cacc[:, :nq],
                                                                   in1=ps[b][:, 2:ncol], op=ALU.mult),
                             r=['cacc', psn[b]], w=['oconv'])
                        yield
                    K.op('act', lambda e: e.activation(out=ocsq[:, :, :nq], in_=oconv[:, :, :nq], func=AF.Square),
                         r=['oconv'], w=[f'xn2.{c_}' for c_ in range(4)])
                    yield
                    b = nxt(*BG)
                    for f in range(4):
                        K.op('pe', lambda e, f=f: e.matmul(ps[b][:, :nq], lhsT=onesb[:], rhs=ocsq[:, f, :nq],
                                                           start=(f == 0), stop=(f == 3)), r=[f'xn2.{f}', 'onesb'], w=[psn[b]], inc=(f == 3))
                    yield
                    rstd_from(ps[b][:, :nq], psn[b], 512.0, oc_rstd[:, :nq], 'ccsb')
                    yield
                    for f in range(4):
                        K.op('dve', lambda e, f=f: e.scalar_tensor_tensor(
                            out=mixedT[:, 4 + f, :nq], in0=oconv[:, f, :nq], scalar=pp[:, PP_OCG + f:PP_OCG + f + 1],
                            in1=oc_rstd[:, :nq], op0=ALU.mult, op1=ALU.mult),
                             r=['oconv', 'ccsb', 'pp'], w=[mxn + 'c'])
                    yield

                def att(k):
                    T, q_lo, nq, j, main = slots[k]
                    xs, csq, ibt, cct, Qp, QA, Gt, mixedT, sfx = bufset(k)
                    n4 = 4 * nq
                    Qpn = f'Qp{sfx}'; QAn = f'QA{sfx}'; ibn = f'ibt{sfx}'; ccn = f'cct{sfx}'
                    ncmp = T // 16 + 1
                    PD = 2

                    def bc4(ap):
                        return ap.unsqueeze(1).to_broadcast([128, 4, nq])
                    its = [(c, g) for c in range(ncmp) for g in range(2)]
                    pend = {}

                    def cmp_s1(i):
                        c, g = its[i]
                        bS = nxt(4, 6)
                        K.op('pe', lambda e: e.matmul(ps[bS][:, :n4], lhsT=kcmpT[64 * g:64 * g + 64, c * 128:(c + 1) * 128],
                                                      rhs=Qp[64 * g:64 * g + 64, :n4], start=True, stop=False),
                             r=['kcmpT', Qpn], w=[psn[bS]])
                        K.op('pe', lambda e: e.matmul(v4(ps[bS][:, :n4]), lhsT=identb[:], rhs=bc4(cct[:, c, :nq]),
                                                      start=False, stop=True), r=['identb', ccn], w=[psn[bS]])
                        ET, etn = next_et()
                        K.op('act', lambda e: e.activation(out=ET[:, :n4], in_=ps[bS][:, :n4], func=AF.Exp),
                             r=[psn[bS]], w=[etn])
                        pend[i] = (ET, etn)
                        pump()

                    def cmp_s2(i):
                        c, g = its[i]
                        ET, etn = pend.pop(i)
                        for r in range(4):
                            bank = g * 2 + r // 2
                            off = (r % 2) * 193
                            K.op('pe', lambda e, r=r: e.matmul(
                                ps[bank][:nq, off:off + 193], lhsT=ET[:, r * nq:(r + 1) * nq], rhs=vcA[:, c, g, :],
                                start=(c == 0 and r % 2 == 0), stop=(c == ncmp - 1), skip_group_check=True),
                                 r=[etn, 'vcA'], w=[psn[bank]])
                    for i in range(len(its) + PD):
                        if i < len(its):
                            cmp_s1(i)
                        if i - PD >= 0:
                            cmp_s2(i - PD)
                    for bk in range(4):
                        K.op('act', lambda e, bk=bk: e.activation(
                            out=OC[:nq, 2 * bk:2 * bk + 2, :].rearrange("p a b -> p (a b)"), in_=ps[bk][:nq, 0:386],
                            func=AF.Copy), r=[psn[bk]], w=['OC'])
                    pump()

                    def attn(tiles, kind, Oout, oname, g_list=(0, 1)):
                        its2 = [(tau, g) for tau in tiles for g in g_list]
                        pend2 = {}
                        Vt = VsA if kind == 's' else VwA
                        vtn = 'VsA' if kind == 's' else 'VwA'

                        def s1(i):
                            tau, g = its2[i]
                            bS = nxt(2, 6)
                            static = None
                            if tau == T:
                                static = (tc_b, 'tc_b')
                            elif kind == 'w' and tau == T - 4:
                                static = (tb_b, 'tb_b')
                            if kind == 's':
                                h = 0 if tau < 32 else 1
                                K.op('pe', lambda e: e.matmul(ps[bS][:, :n4], lhsT=ksA[g][:, tau * 128:(tau + 1) * 128],
                                                              rhs=QA[g][h][:, :n4], start=True, stop=(static is None)),
                                     r=[f'ksA{g}', QAn + f'{g}{h}q', QAn + f'{g}{h}b'], w=[psn[bS]])
                            else:
                                K.op('pe', lambda e: e.matmul(ps[bS][:, :n4],
                                                              lhsT=kwT[64 * g:64 * g + 64, tau * 128:(tau + 1) * 128],
                                                              rhs=Qp[64 * g:64 * g + 64, :n4], start=True,
                                                              stop=(static is None)),
                                     r=['kwT', Qpn], w=[psn[bS]])
                            if static is not None:
                                K.op('pe', lambda e: e.matmul(v4(ps[bS][:, :n4]), lhsT=identb[:],
                                                              rhs=bc4(static[0][:, q_lo:q_lo + nq]), start=False, stop=True),
                                     r=['identb', static[1]], w=[psn[bS]])
                            ET, etn = next_et()
                            if tau < 3:
                                K.op('act', lambda e: e.activation(out=ET[:, :n4], in_=ps[bS][:, :n4], func=AF.Exp,
                                                                   bias=pp[:, PP_PAD + tau:PP_PAD + tau + 1]),
                                     r=[psn[bS], 'pp'], w=[etn])
                            else:
                                K.op('act', lambda e: e.activation(out=ET[:, :n4], in_=ps[bS][:, :n4], func=AF.Exp),
                                     r=[psn[bS]], w=[etn])
                            pend2[i] = (ET, etn)

                        def s2(i):
                            tau, g = its2[i]
                            ET, etn = pend2.pop(i)
                            K.op('pe', lambda e: e.matmul(ps[g][:65, :n4], lhsT=Vt[:, tau, g, :], rhs=ET[:, :n4],
                                                          start=(tau == tiles[0]), stop=(tau == tiles[-1])),
                                 r=[etn, vtn], w=[psn[g]])
                            pump()
                        for i in range(len(its2) + PD):
                            if i < len(its2):
                                s1(i)
                            if i - PD >= 0:
                                s2(i - PD)
                        OT = oatt[:].rearrange("p a b -> p (a b)")
                        for g in g_list:
                            K.op('pool', lambda e: e.memset(OT[64:66, :n4], 0.0), w=['oatt'])
                            K.op('act', lambda e, g=g: e.activation(out=OT[:65, :n4], in_=ps[g][:65, :n4], func=AF.Copy),
                                 r=[psn[g]], w=['oatt'])
                            bT = nxt(2, 6)
                            for r in range(4):
                                K.op('pe', lambda e, r=r: e.transpose(out=ps[bT][:nq, r * 66:(r + 1) * 66],
                                                                      in_=OT[:66, r * nq:(r + 1) * nq],
                                                                      identity=identf[:66, :66]),
                                     r=['oatt', 'identf'], w=[psn[bT]])
                            K.op('act', lambda e, g=g: e.activation(
                                out=Oout[:nq, 4 * g:4 * g + 4, :],
                                in_=ps[bT][:nq, 0:264].rearrange("p (a b) -> p a b", a=4)[:, :, 0:65],
                                func=AF.Copy), r=[psn[bT]], w=[oname])
                            pump()

                    K.op('dve', lambda e: e.tensor_scalar(out=rsm[:nq, 0, :], in0=OC[:nq, :, 64], scalar1=1e-30, scalar2=None,
                                                          op0=ALU.max), r=['OC'], w=['rsm'])
                    K.op('dve', lambda e: e.reciprocal(out=rsm[:nq, 0, :], in_=rsm[:nq, 0, :]), r=['rsm'], w=['rsm'])
                    for g in range(2):
                        h0 = 4 * g
                        biasq = biasq2[g]; biasw = biasw2[g]; bqn = f'biasq{g}'; bwn = f'biasw{g}'
                        K.op('dve', lambda e: e.tensor_scalar(out=impb[:nq, :], in0=OC[:nq, h0, 65:193],
                                                              scalar1=rsm[:nq, 0, h0:h0 + 1], scalar2=None, op0=ALU.mult),
                             r=['OC', 'rsm'], w=['impb'])
                        for r in range(1, 4):
                            K.op('dve', lambda e, r=r: e.scalar_tensor_tensor(
                                out=impb[:nq, :], in0=OC[:nq, h0 + r, 65:193], scalar=rsm[:nq, 0, h0 + r:h0 + r + 1],
                                in1=impb[:nq, :], op0=ALU.mult, op1=ALU.add), r=['OC', 'rsm', 'impb'], w=['impb'])
                        K.op('dve', lambda e: e.tensor_tensor(out=impb[:nq, :], in0=impb[:nq, :], in1=ibt[:nq, :], op=ALU.add),
                             r=['impb', ibn], w=['impb'])
                        pump()
                        K.op('dve', lambda e: e.max(out=m8a[:nq, :], in_=impb[:nq, :]), r=['impb'], w=['m8a'])
                        K.op('dve', lambda e: e.match_replace(out=mscr[:nq, :], in_to_replace=m8a[:nq, :],
                                                              in_values=impb[:nq, :], imm_value=-9e9),
                             r=['impb', 'm8a'], w=['mscr'])
                        K.op('dve', lambda e: e.max(out=m8b[:nq, :], in_=mscr[:nq, :]), r=['mscr'], w=['m8b'])
                        pump()
                        K.op('dve', lambda e: e.tensor_scalar(out=mscr[:nq, :], in0=impb[:nq, :], scalar1=m8b[:nq, 7:8],
                                                              scalar2=None, op0=ALU.is_ge), r=['impb', 'm8b', 'mscr'], w=['mscr'])
                        K.op('dve', lambda e: e.tensor_scalar(out=biasq[:nq, :], in0=mscr[:nq, :], scalar1=1.0, scalar2=BIG,
                                                              op0=ALU.subtract, op1=ALU.mult), r=['mscr'], w=[bqn])
                        K.op('pool', lambda e: e.tensor_copy(out=biasw[:nq, 0:64], in_=biasq[:nq, 64:128]),
                             r=[bqn], w=[bwn])
                        K.op('pool', lambda e: e.tensor_copy(out=biasw[:nq, 64:128], in_=biasq[:nq, 0:64]),
                             r=[bqn, bwn], w=[bwn])
                        pump()
                    attn(list(range(max(T - 4, 0), T + 1)), 'w', OW, 'OW')
                    for g in range(2):
                        biasq = biasq2[g]; biasw = biasw2[g]; bqn = f'biasq{g}'; bwn = f'biasw{g}'
                        bT = nxt(4, 6)
                        K.op('pe', lambda e: e.transpose(out=ps[bT][:, 0:nq], in_=biasq[:nq, :], identity=identf[:nq, :nq]),
                             r=[bqn, 'identf'], w=[psn[bT]])
                        K.op('pe', lambda e: e.transpose(out=ps[bT][:, 128:128 + nq], in_=biasw[:nq, :],
                                                         identity=identf[:nq, :nq]), r=[bwn, 'identf'], w=[psn[bT]])
                        if g == 0:
                            rows = slice(64, 128); lo_src = ps[bT][64:128, 128:128 + nq]; hi_src = ps[bT][64:128, 0:nq]
                        else:
                            rows = slice(0, 64); lo_src = ps[bT][0:64, 0:nq]; hi_src = ps[bT][0:64, 128:128 + nq]
                        for h, src in ((0, lo_src), (1, hi_src)):
                            K.op('dve', lambda e, h=h, src=src: e.tensor_copy(
                                out=v4(QA[g][h][rows, :n4]), in_=src.unsqueeze(1).to_broadcast([64, 4, nq])),
                                 r=[psn[bT]], w=[QAn + f'{g}{h}b'])
                        pump()
                    attn(list(range(T + 1)), 's', OS, 'OS')

                def post_a(k):
                    T, q_lo, nq, j, main = slots[k]
                    xs, csq, ibt, cct, Qp, QA, Gt, mixedT, sfx = bufset(k)
                    Gtn = f'Gt{sfx}'
                    for br, O_, on in ((1, OS, 'OS'), (2, OW, 'OW')):
                        K.op('dve', lambda e, br=br, O_=O_: e.tensor_scalar(out=rsm[:nq, br, :], in0=O_[:nq, :, 64],
                                                                            scalar1=1e-30, scalar2=None, op0=ALU.max),
                             r=[on], w=['rsm'])
                        K.op('dve', lambda e, br=br: e.reciprocal(out=rsm[:nq, br, :], in_=rsm[:nq, br, :]),
                             r=['rsm'], w=['rsm'])
                    K.op('dve', lambda e: e.tensor_tensor(out=coef[:nq, :, :], in0=rsm[:nq, :, :],
                                                          in1=Gt[:nq, :].rearrange("p (h b) -> p b h", b=3), op=ALU.mult),
                         r=['rsm', Gtn], w=['coef'])

                    def cbc(br):
                        return coef[:nq, br, :].unsqueeze(2).to_broadcast([nq, 8, 64])
                    K.op('dve', lambda e: e.tensor_tensor(out=oatt[:nq], in0=OC[:nq, :, 0:64], in1=cbc(0), op=ALU.mult),
                         r=['OC', 'coef'], w=['oatt'])
                    K.op('pool', lambda e: e.tensor_tensor(out=otmp[:nq], in0=OS[:nq, :, 0:64], in1=cbc(1), op=ALU.mult),
                         r=['OS', 'coef'], w=['otmp'])
                    K.op('dve', lambda e: e.tensor_tensor(out=oatt[:nq], in0=oatt[:nq], in1=otmp[:nq], op=ALU.add),
                         r=['oatt', 'otmp'], w=['oatt'])
                    K.op('pool', lambda e: e.tensor_tensor(out=otmp[:nq], in0=OW[:nq, :, 0:64], in1=cbc(2), op=ALU.mult),
                         r=['OW', 'coef', 'oatt'], w=['otmp'])
                    K.op('dve', lambda e: e.tensor_tensor(out=oatt[:nq], in0=oatt[:nq], in1=otmp[:nq], op=ALU.add),
                         r=['oatt', 'otmp'], w=['oatt'])
                    oflat = oatt[:nq].rearrange("p a b -> p (a b)")
                    K.op('act', lambda e: e.activation(out=o_n[:nq, :], in_=oflat, func=AF.Square, accum_out=ssq[:nq, :]),
                         r=['oatt'], w=['otmp', 'ssq'])
                    K.op('act', lambda e: e.activation(out=ssl[:nq, :], in_=ssq[:nq, :], func=AF.Ln, bias=epsb[:nq, :],
                                                       scale=1.0 / 512.0), r=['ssq', 'epsb'], w=['ssl'])
                    K.op('act', lambda e: e.activation(out=ssr[:nq, :], in_=ssl[:nq, :], func=AF.Exp, scale=-0.5),
                         r=['ssl'], w=['ssr'])
                    K.op('dve', lambda e: e.tensor_scalar(out=o_n[:nq, :], in0=oflat, scalar1=ssr[:nq, 0:1], scalar2=None,
                                                          op0=ALU.mult), r=['oatt', 'ssr', 'otmp'], w=['otmp'])

                def post_b_g(k):
                    T, q_lo, nq, j, main = slots[k]
                    xs, csq, ibt, cct, Qp, QA, Gt, mixedT, sfx = bufset(k)
                    ncol = nq + 2
                    n4 = 4 * nq
                    xsn = f'xs{sfx}'; mxn = f'mixedT{sfx}'
                    bT = nxt(*BG)
                    for f in range(4):
                        K.op('pe', lambda e, f=f: e.transpose(out=ps[bT][:, f * nq:(f + 1) * nq],
                                                              in_=o_n[:nq, f * 128:(f + 1) * 128], identity=identf[:nq, :nq]),
                             r=['otmp', 'identf'], w=[psn[bT]])
                    yield
                    for f in range(4):
                        K.op('dve', lambda e, f=f: e.tensor_scalar(out=mixedT[:, f, :nq], in0=ps[bT][:, f * nq:(f + 1) * nq],
                                                                   scalar1=pp[:, PP_OAG + f:PP_OAG + f + 1], scalar2=None,
                                                                   op0=ALU.mult), r=[psn[bT], 'pp'], w=[mxn + 'a'])
                        if f % 2 == 1:
                            yield
                    for half in range(2):
                        b = nxt(*BG)
                        for mm in range(4):
                            m = half * 4 + mm
                            for kc in range(8):
                                K.op('pe', lambda e, m=m, mm=mm, kc=kc: e.matmul(
                                    ps[b][:, mm * nq:(mm + 1) * nq], lhsT=wo[:, kc, m * 128:(m + 1) * 128],
                                    rhs=mixedT[:, kc, :nq], start=(kc == 0), stop=(kc == 7)),
                                     r=['wo', mxn + 'a', mxn + 'c'], w=[psn[b]], inc=(kc == 7))
                            yield
                        K.op('dve', lambda e: e.tensor_tensor(out=xs[:, half * 4:half * 4 + 4, 2:ncol], in0=v4(ps[b][:, :n4]),
                                                              in1=xs[:, half * 4:half * 4 + 4, 2:ncol], op=ALU.add),
                             r=[psn[b], xsn], w=[xsn])
                        yield
                    if main:
                        K.dma('sp', hmd[j][:, :, 2:130], xs[:, :, 2:130], r=[xsn], w=['hmd'], key='hmd')
                    yield
                    yield from rmsnorm_fm_g(xs[:, :, 2:ncol], xsn, nq, PP_GFFN, xn2, 'xn2', BG)
                    if main:
                        K.dma('sp', hn3d[j][:, :, 2:130], xn2[:, :, 0:128], r=[f'xn2.{c_}' for c_ in range(8)], w=['hn3d'], key='hn3d')
                    else:
                        K.dma('sp', hn3d[j][:, :, 0:2], xn2[:, :, nq - 2:nq], r=[f'xn2.{c_}' for c_ in range(8)], w=['hn3d'], key='hn3d')
                    yield

                run(pre_g(0))
                run(pre_g(1))
                for j in range(NSLOT):
                    kh, km = 2 * j, 2 * j + 1
                    ensure(('pre', kh))
                    if j >= 1:
                        bgq.append((('postb', km - 2), post_b_g(km - 2)))
                    pmul['n'] = 1
                    att(kh)
                    pmul['n'] = 1
                    if j >= 1:
                        ensure(('postb', km - 2))
                    post_a(kh)
                    ensure(('pre', km))
                    bgq.append((('postb', kh), post_b_g(kh)))
                    if j + 1 < NSLOT:
                        bgq.append((('pre', kh + 2), pre_g(kh + 2)))
                        bgq.append((('pre', km + 2), pre_g(km + 2)))
                    att(km)
                    ensure(('postb', kh))
                    post_a(km)
                    if stop == 'p2s' and j == int(os.environ.get('KSLOT', '0')):
                        k = km
                        while bgq:
                            pump()
                        run(post_b_g(k))
                        K.barrier()
                        dump('hm', bufset(k)[0][:].rearrange("p a b -> p (a b)"), f'xs{bufset(k)[8]}')
                        dump('oatt', oatt[:].rearrange("p a b -> p (a b)"), 'oatt')
                        stop_here()
                while bgq:
                    pump()
                run(post_b_g(NSL - 1))

        K.barrier()
        with ExitStack() as es3:
            sb3 = mk(es3)
            hacc = sb3("hacc", [128, NSLOT, 8, 128])
            fence = sb3("fence", [128, 1])
            hn3 = sb3("hn3", [128, NSLOT, 8, 130], BF16)
            es3w = ExitStack()
            sb3w = mk(es3w)
            PASSES = [(0, 4), (4, 10), (10, 16), (16, 22)]
            wugs = [sb3w(f"wug{i}", [128, 8, 768], BF16) for i in range(2)]
            wuus = [sb3w(f"wuu{i}", [128, 8, 768], BF16) for i in range(2)]
            wdns = [sb3w(f"wdn{i}", [128, 6, 1024], BF16) for i in range(2)]
            actTs = [sb3w(f"actT{i}", [128, 6, 128], BF16) for i in range(2)]
            NFA = 4
            faccs = [sb3w(f"facc{i}", [128, 128]) for i in range(NFA)]
            fss = [sb3w(f"fs{i}", [128, 128]) for i in range(NFA)]
            fus = [sb3w(f"fu{i}", [128, 128]) for i in range(NFA)]

            def load_pass(p):
                c0, c1 = PASSES[p]
                bi = p % 2
                load_w(wugs[bi], w_up, 8, c0 * 128, c1 * 128, f'wug{bi}')
                load_w(wuus[bi], w_up, 8, DFF + c0 * 128, DFF + c1 * 128, f'wuu{bi}')
                for f in range(c1 - c0):
                    K.dma('pool', wdns[bi][:, f, :], w_down[(c0 + f) * 128:(c0 + f + 1) * 128, :], w=[f'wdn{bi}'],
                          key=f'wdn{bi}')
            load_pass(0)
            for j in range(NSLOT):
                K.dma('sp', hn3[:, j], hn3d[j], r=['hn3d'], w=['hn3'], key='hn3')
                K.dma('sp', hacc[:, j], hmd[j][:, :, 2:130], r=['hmd'], w=['hacc_all'], key='hacc')
            K.op('dve', lambda e: e.memset(fence[:], 0.0), r=['hacc_all'], w=[f'hacc{j}' for j in range(NSLOT)])
            fi = {'i': 0}
            for p in range(len(PASSES)):
                c0, c1 = PASSES[p]
                ncf = c1 - c0
                bi = p % 2
                wug, wuu, wdn = wugs[bi], wuus[bi], wdns[bi]
                if p + 1 < len(PASSES):
                    load_pass(p + 1)

                def down(j, ncf=ncf, wdn=wdn, bi=bi):
                    aT = actTs[j % 2]; an = f'actT{j % 2}'
                    for half in range(2):
                        b = nxt()
                        for mm in range(4):
                            m = half * 4 + mm
                            for f in range(ncf):
                                K.op('pe', lambda e, m=m, mm=mm, f=f: e.matmul(
                                    ps[b][:, mm * 128:(mm + 1) * 128], lhsT=wdn[:, f, m * 128:(m + 1) * 128],
                                    rhs=aT[:, f, :], start=(f == 0), stop=(f == ncf - 1)),
                                     r=[f'wdn{bi}', an], w=[psn[b]], inc=(f == ncf - 1))
                        K.op('dve', lambda e, half=half: e.tensor_tensor(
                            out=hacc[:, j, half * 4:half * 4 + 4, :], in0=ps[b][:].rearrange("p (a n) -> p a n", a=4),
                            in1=hacc[:, j, half * 4:half * 4 + 4, :], op=ALU.add), r=[psn[b], f'hacc{j}'], w=[f'hacc{j}'])
                for j in range(NSLOT):
                    aT = actTs[j % 2]; an = f'actT{j % 2}'
                    for f in range(ncf):
                        F = c0 + f
                        bg = nxt()
                        for kc in range(8):
                            K.op('pe', lambda e, kc=kc: e.matmul(ps[bg][:, 0:130], lhsT=wug[:, kc, f * 128:(f + 1) * 128],
                                                                 rhs=hn3[:, j, kc, :], start=(kc == 0), stop=(kc == 7)),
                                 r=[f'wug{bi}', 'hn3'], w=[psn[bg]], inc=(kc == 7))
                        for kc in range(8):
                            K.op('pe', lambda e, kc=kc: e.matmul(ps[bg][:, 130:260], lhsT=wuu[:, kc, f * 128:(f + 1) * 128],
                                                                 rhs=hn3[:, j, kc, :], start=(kc == 0), stop=(kc == 7)),
                                 r=[f'wuu{bi}', 'hn3'], w=[psn[bg]], inc=(kc == 7))
                        k = fi['i'] % NFA
                        fi['i'] += 1
                        facc = faccs[k]; fs = fss[k]; fu = fus[k]
                        fan = f'facc{k}'; fsn = f'fs{k}'; fun = f'fu{k}'
                        fw = lambda kk: pp[:, PP_FW + 3 * F + kk:PP_FW + 3 * F + kk + 1]
                        K.op('act', lambda e: e.activation(out=facc[:], in_=ps[bg][:, 2:130], func=AF.Identity,
                                                           bias=pp[:, PP_FB + F:PP_FB + F + 1], scale=fw(2)),
                             r=[psn[bg], 'pp'], w=[fan])
                        K.op('act', lambda e: e.activation(out=fu[:], in_=ps[bg][:, 132:260], func=AF.Copy),
                             r=[psn[bg]], w=[fun])
                        K.op('dve', lambda e: e.scalar_tensor_tensor(out=facc[:], in0=ps[bg][:, 1:129], scalar=fw(1),
                                                                     in1=facc[:], op0=ALU.mult, op1=ALU.add),
                             r=[psn[bg], 'pp', fan], w=[fan])
                        K.op('dve', lambda e: e.scalar_tensor_tensor(out=facc[:], in0=ps[bg][:, 0:128], scalar=fw(0),
                                                                     in1=facc[:], op0=ALU.mult, op1=ALU.add),
                             r=[psn[bg], 'pp', fan], w=[fan])
                        K.op('act', lambda e: e.activation(out=fs[:], in_=facc[:], func=AF.Silu), r=[fan], w=[fsn])
                        K.op('dve', lambda e, f=f: e.tensor_tensor(out=aT[:, f, :], in0=fs[:], in1=fu[:], op=ALU.mult),
                             r=[fsn, fun], w=[an])
                        if f == 1 and j > 0:
                            down(j - 1)
                down(NSLOT - 1)
            K.barrier()
            es3w.close()
            wpg = sb3("wpg", [128, 8, 1024], BF16); wpe = sb3("wpe", [128, 2, 1024], BF16)
            hn4s = [sb3(f"hn4{i}", [128, 8, 128], BF16) for i in range(2)]
            pt2 = [sb3(f"pt{i}", [128, 2, 128]) for i in range(2)]
            ptbs = [sb3(f"ptb{i}", [128, 2, 128], BF16) for i in range(2)]
            pe_es = [sb3(f"pe_e{i}", [128, 512]) for i in range(2)]
            pe_ts = [sb3(f"pe_t{i}", [128, 512]) for i in range(2)]
            ob2 = [sb3(f"ob{i}", [128, 8, 128]) for i in range(2)]
            load_w(wpg, w_pg, 8, 0, 1024, 'wpg'); load_w(wpe, w_pe, 2, 0, 1024, 'wpe')

            def pleA_g(j):
                s = j % 2
                K.dma('sp', pt2[s][:], ptd[j], w=[f'pt{s}'], key=f'pt{s}')
                K.op('pool', lambda e: e.tensor_copy(out=ptbs[s][:], in_=pt2[s][:]), r=[f'pt{s}'], w=[f'ptb{s}'])
                yield
                yield from rmsnorm_fm_g(hacc[:, j], f'hacc{j}', 128, PP_GPLE, hn4s[s], f'hn4{s}', (6, 8))

            def pleB_g(j):
                s = j % 2
                ob = ob2[s]; hn4 = hn4s[s]; ptb = ptbs[s]
                for half in range(2):
                    pe_e = pe_es[half]; pe_t = pe_ts[half]; pen = f'pe_e{half}'; ptn = f'pe_t{half}'
                    b1 = nxt(0, 6); b2 = nxt(0, 6)
                    for mm in range(4):
                        m = half * 4 + mm
                        for kc in range(8):
                            K.op('pe', lambda e, m=m, mm=mm, kc=kc: e.matmul(
                                ps[b1][:, mm * 128:(mm + 1) * 128], lhsT=wpg[:, kc, m * 128:(m + 1) * 128],
                                rhs=hn4[:, kc, :], start=(kc == 0), stop=(kc == 7)),
                                 r=['wpg', f'hn4{s}.{kc}'], w=[psn[b1]], inc=(kc == 7))
                        for kc in range(2):
                            K.op('pe', lambda e, m=m, mm=mm, kc=kc: e.matmul(
                                ps[b2][:, mm * 128:(mm + 1) * 128], lhsT=wpe[:, kc, m * 128:(m + 1) * 128],
                                rhs=ptb[:, kc, :], start=(kc == 0), stop=(kc == 1)),
                                 r=['wpe', f'ptb{s}'], w=[psn[b2]], inc=(kc == 1))
                        yield
                    K.op('act', lambda e: e.activation(out=pe_e[:], in_=ps[b1][:], func=AF.Sigmoid),
                         r=[psn[b1]], w=[pen])
                    yield
                    K.op('dve', lambda e: e.tensor_tensor(out=pe_t[:], in0=pe_e[:], in1=ps[b2][:], op=ALU.mult),
                         r=[pen, psn[b2]], w=[ptn])
                    yield
                    K.op('dve', lambda e, half=half: e.tensor_tensor(
                        out=ob[:, half * 4:half * 4 + 4, :], in0=pe_t[:].rearrange("p (a n) -> p a n", a=4),
                        in1=hacc[:, j, half * 4:half * 4 + 4, :], op=ALU.add), r=[ptn, f'hacc{j}'], w=[f'ob{s}'])
                    yield
                K.dma('sp', outT[j], ob[:], r=[f'ob{s}'], w=[f'outd{s}'], key=f'out{s}')
                yield

            run(pleA_g(0))
            for j in range(NSLOT):
                gens = [pleB_g(j)]
                if j + 1 < NSLOT:
                    gens.append(pleA_g(j + 1))
                while gens:
                    for g_ in list(gens):
                        try:
                            next(g_)
                        except StopIteration:
                            gens.remove(g_)
            K.finish('sp', ['out0', 'out1'])
    return nc


_NC_CACHE = {}


def _core_tables(r):
    pad = 3 - r
    half = 8
    inv_freq = (ROPE_THETA ** (-np.arange(half, dtype=np.float32) * 2.0 / 16.0)).astype(np.float32)

    def cs_tables(pos):
        ang = pos.astype(np.float32)[None, :] * inv_freq[:, None]
        c = np.ones((64, pos.shape[0]), np.float32)
        s = np.zeros((64, pos.shape[0]), np.float32)
        c[0:8] = np.cos(ang); c[8:16] = np.cos(ang)
        s[0:8] = np.sin(ang); s[8:16] = np.sin(ang)
        return np.concatenate([c, c], 0), np.concatenate([s, s], 0)
    pos = np.arange(SEQ) - 128 * pad
    cosT, sinT = cs_tables(pos)
    cpos = 16 * (np.arange(512) - 8 * pad) + 31
    cosC, sinC = cs_tables(cpos)
    cc = np.full((2 * NSLOT, 4, 128, 128), -BIG, np.float32)
    ib = np.zeros((2 * NSLOT, 128, 128), np.float32)
    b0 = 2 * pad
    blk = np.arange(128)
    for j in range(NSLOT):
        for main in range(2):
            ti = 2 * j + main
            T = 4 * j + 2 + main
            tq = 128 * T + np.arange(128)
            for c in range(4):
                n_p = 128 * c + np.arange(128)
                valid = (16 * n_p[:, None] + 31 <= tq[None, :]) & (n_p[:, None] >= 8 * pad)
                cc[ti, c] = np.where(valid, 0.0, -BIG)
            cur = tq // 64
            v = np.zeros((128, 128), np.float32)
            B = blk[None, :]
            C = cur[:, None]
            v = np.where(B > C, -1e9 - B * 1e6, v)
            v = np.where(B == b0, 1e9, v)
            v = np.where(B == C - 1, 2e9, v)
            v = np.where(B == C, 3e9, v)
            v = np.where(B < b0, -3e9 - B * 1e6, v)
            ib[ti] = v
    padb = np.zeros((128, 4), np.float32)
    for t in range(3):
        if t < pad:
            padb[:, t] = -BIG
    return cosT, sinT, cosC, sinC, cc, ib, padb


def _prep(x, p, ln_mix_g, w_in, qn_g, kn_g, pe_k, pe_v, w_ck1, w_ck2, w_cv1, w_cv2,
           conv_w, on_att_g, on_conv_g, w_o, ln_ffn_g, w_up, ffn_conv_w, ffn_conv_b, w_down,
           ln_ple_g, w_pg, w_pe):
    f32 = np.float32
    x = np.asarray(x, f32); p = np.asarray(p, f32)
    w_in0 = np.asarray(w_in, f32)[0]
    qperm = np.array([g * 256 + r * 64 + d for r in range(4) for g in range(2) for d in range(64)])
    cols = np.concatenate([qperm, np.arange(512, 640), np.arange(640, 768), np.arange(768, 896),
                           np.arange(1024, 1152), np.arange(896, 1024), np.arange(1152, 1280),
                           np.arange(1280, 2840)])
    w_in_p = np.ascontiguousarray(w_in0[:, cols])

    def colpack(v, n):
        return np.asarray(v, f32).reshape(n, 128).T
    pp_base = np.zeros((128, NPP), f32)
    pp_base[:, PP_GMIX:PP_GMIX + 8] = colpack(ln_mix_g[0], 8)
    pp_base[:, PP_GFFN:PP_GFFN + 8] = colpack(ln_ffn_g[0], 8)
    pp_base[:, PP_GPLE:PP_GPLE + 8] = colpack(ln_ple_g[0], 8)
    pp_base[:, PP_OAG:PP_OAG + 4] = colpack(on_att_g[0], 4)
    pp_base[:, PP_OCG:PP_OCG + 4] = colpack(on_conv_g[0], 4)
    pp_base[:, PP_QG] = np.tile(np.asarray(qn_g, f32)[0], 2)
    for b in range(3):
        pp_base[:, PP_KG + b] = np.tile(np.asarray(kn_g, f32)[0, b], 2)
    cw = np.asarray(conv_w, f32)[0]
    for f in range(4):
        for k in range(3):
            pp_base[:, PP_CW + 3 * f + k] = cw[k, f * 128:(f + 1) * 128]
    fw = np.asarray(ffn_conv_w, f32)[0]
    fb = np.asarray(ffn_conv_b, f32)[0]
    for F in range(NF):
        for k in range(3):
            pp_base[:, PP_FW + 3 * F + k] = fw[k, F * 128:(F + 1) * 128]
        pp_base[:, PP_FB + F] = fb[F * 128:(F + 1) * 128]

    def w1pack(w):
        a = np.asarray(w, f32)[0].transpose(1, 0, 2).reshape(64, 32 * 256)
        return np.ascontiguousarray(np.concatenate([a, a], 0))

    def w2pack(w):
        a = np.asarray(w, f32)[0].reshape(2, 128, 64).transpose(1, 0, 2).reshape(128, 128)
        return np.ascontiguousarray(a)

    def pepack(pe):
        a = np.asarray(pe, f32)[0].T
        return np.ascontiguousarray(np.concatenate([a, a], 0))
    kk = np.arange(128)
    tcd = np.where(kk[:, None] <= kk[None, :], 0.0, -BIG).astype(f32)
    tbd = np.where(kk[:, None] > kk[None, :], 0.0, -BIG).astype(f32)
    keys = np.arange(SEQ)
    indd = (((keys[None, :] // 64) % 64) == np.arange(64)[:, None]).astype(f32)
    n_p = np.arange(512)
    sblk = np.arange(128)
    ov = np.clip(np.minimum(16 * n_p[:, None] + 32, 64 * sblk[None, :] + 64)
                 - np.maximum(16 * n_p[:, None], 64 * sblk[None, :]), 0, None).astype(f32) / 32.0
    cmd = np.ascontiguousarray(ov.reshape(4, 128, 128).transpose(1, 0, 2))
    rotd = np.zeros((128, 128), f32)
    obdd = np.zeros((128, 128), f32)
    for g in range(2):
        o = 64 * g
        obdd[o:o + 64, o:o + 64] = 1.0
        for m in range(8):
            rotd[o + m + 8, o + m] = -1.0
            rotd[o + m, o + m + 8] = 1.0
    shared = dict(w_in=w_in_p, w_o=np.asarray(w_o, f32)[0], w_up=np.asarray(w_up, f32)[0],
                  w_down=np.asarray(w_down, f32)[0], w_pg=np.asarray(w_pg, f32)[0], w_pe=np.asarray(w_pe, f32)[0],
                  w1k=w1pack(w_ck1), w1v=w1pack(w_cv1), w2k=w2pack(w_ck2), w2v=w2pack(w_cv2),
                  pek=pepack(pe_k), pev=pepack(pe_v), tcd=tcd, tbd=tbd, indd=indd, cmd=cmd, rotd=rotd, obdd=obdd)
    tabs = [_core_tables(r) for r in range(4)]
    in_maps = []
    for c in range(8):
        b, r = c // 4, c % 4
        pad = 3 - r
        xprog = np.zeros((SEQ, D), f32)
        xprog[128 * pad:] = x[b, :SEQ - 128 * pad]
        xts = np.ascontiguousarray(xprog.reshape(NT, 128, 8, 128).transpose(0, 3, 2, 1))
        pb = p[0, b].reshape(NT, 128, 2, 128)
        ptd = np.ascontiguousarray(pb[r::4].transpose(0, 3, 2, 1))
        cosT, sinT, cosC, sinC, cc, ib, padb = tabs[r]
        ppc = pp_base.copy()
        ppc[:, PP_PAD:PP_PAD + 4] = padb
        m = dict(shared)
        m.update(xts=xts, ptd=ptd, cosT=cosT, sinT=sinT, cosC=cosC, sinC=sinC, ccd=cc, ibd=ib, ppd=ppc)
        in_maps.append(m)
    return in_maps


def kernel(**inputs):
    f32 = np.float32
    in_maps = _prep(**inputs)
    if 'nc' not in _NC_CACHE:
        _NC_CACHE['nc'] = build_nc()
    res = run_bass_kernel_spmd(_NC_CACHE['nc'], in_maps, core_ids=list(range(8)))
    out = np.zeros((2, SEQ, D), f32)
    for c in range(8):
        b, r = c // 4, c % 4
        o = np.asarray(res.results[c]["outT"], f32)
        o = o.transpose(0, 3, 2, 1).reshape(NSLOT, 128, D)
        out[b].reshape(NT, 128, D)[r::4] = o
    return out
```
